# Optimizing a Trainium2 kernel written in Bass

```python
import jax
import jax.numpy as jnp
from jax import lax
import numpy as np

D_MODEL = 1024
BATCH = 8
SEQ = 4096
DEPTH = 1

MIX_WIDTH = D_MODEL
DN_HEAD_DIM = 128
DN_WIDTH = MIX_WIDTH // 2
DN_HEADS = DN_WIDTH // DN_HEAD_DIM
DN_CONV = 4
DN_CHUNK = 64
SB_HEAD_DIM = 64
SB_WIDTH = MIX_WIDTH - DN_WIDTH
SB_HEADS = SB_WIDTH // SB_HEAD_DIM
SB_BLOCK = 128
IN_SIZES = (DN_WIDTH, DN_WIDTH, DN_WIDTH, DN_WIDTH, DN_HEADS, DN_HEADS, SB_WIDTH, SB_WIDTH, SB_WIDTH)
IN_COLS = sum(IN_SIZES)
PEER_HEADS = 8
PEER_N_KEYS = 128
PEER_EXPERTS = PEER_N_KEYS * PEER_N_KEYS
PEER_QUERY_DIM = 256
PEER_HALF = PEER_QUERY_DIM // 2
PEER_TOPK = 16
PEER_TOKEN_BLOCK = 128
N_MOD = 6
EPS = 1e-6

kernel_name = "hybrid_deltanet_stickbreak_peer_block"


def rms_norm(x, gain):
    xf = x.astype(jnp.float32)
    y = xf * lax.rsqrt(jnp.mean(xf * xf, axis=-1, keepdims=True) + EPS)
    return (y * gain.astype(jnp.float32)).astype(x.dtype)


def l2_norm(x):
    xf = x.astype(jnp.float32)
    return xf * lax.rsqrt(jnp.sum(xf * xf, axis=-1, keepdims=True) + EPS)


def causal_depthwise_conv(u, w):
    ch = u.shape[-1]
    return lax.conv_general_dilated(
        u, w[:, None, :].astype(u.dtype), window_strides=(1,), padding=[(w.shape[0] - 1, 0)],
        dimension_numbers=("NWC", "WIO", "NWC"), feature_group_count=ch)


def gated_delta_rule(q, k, v, g, beta):
    b, h, s, dk = q.shape
    dv = v.shape[-1]
    c = DN_CHUNK
    n = s // c
    q = q.reshape(b, h, n, c, dk)
    k = k.reshape(b, h, n, c, dk)
    v = v.reshape(b, h, n, c, dv)
    g = jnp.cumsum(g.reshape(b, h, n, c), axis=-1)
    beta = beta.reshape(b, h, n, c)[..., None]
    idx = jnp.arange(c)
    lower_incl = idx[:, None] >= idx[None, :]
    lower_strict = idx[:, None] > idx[None, :]
    decay = jnp.exp(jnp.where(lower_incl, g[..., :, None] - g[..., None, :], -jnp.inf))
    kbeta = k * beta
    kk = jnp.einsum('bhnid,bhnjd->bhnij', kbeta, k)
    tri = jnp.eye(c, dtype=jnp.float32) + jnp.where(lower_strict, kk * decay, 0.0)
    rhs = jnp.concatenate([kbeta * jnp.exp(g)[..., None], v * beta], axis=-1)
    sol = lax.linalg.triangular_solve(tri, rhs, left_side=True, lower=True, unit_diagonal=True)
    w_c, u_c = sol[..., :dk], sol[..., dk:]
    attn = jnp.einsum('bhnid,bhnjd->bhnij', q, k) * decay
    q_dec = q * jnp.exp(g)[..., None]
    k_dec = k * jnp.exp(g[..., -1:] - g)[..., None]
    g_last = jnp.exp(g[..., -1])

    def step(state, xs):
        q_i, k_i, w_i, u_i, a_i, gl_i = xs
        v_new = u_i - jnp.einsum('bhcd,bhde->bhce', w_i, state)
        o_i = jnp.einsum('bhcd,bhde->bhce', q_i, state) + jnp.einsum('bhij,bhje->bhie', a_i, v_new)
        state = state * gl_i[..., None, None] + jnp.einsum('bhcd,bhce->bhde', k_i, v_new)
        return state, o_i

    xs = tuple(jnp.moveaxis(t, 2, 0) for t in (q_dec, k_dec, w_c, u_c, attn, g_last))
    state0 = jnp.zeros((b, h, dk, dv), jnp.float32)
    _, o = lax.scan(step, state0, xs)
    return jnp.moveaxis(o, 0, 2).reshape(b, h, s, dv)


def stick_breaking_attention(q, k, v):
    s_len, d = q.shape[2], q.shape[3]
    scale = d ** -0.5
    outs = []
    for i in range(s_len // SB_BLOCK):
        t0 = i * SB_BLOCK
        t1 = t0 + SB_BLOCK
        z = jnp.einsum('bhtd,bhsd->bhts', q[:, :, t0:t1], k[:, :, :t1]) * scale
        causal = jnp.arange(t1)[None, :] < jnp.arange(t0, t1)[:, None]
        log_beta = jax.nn.log_sigmoid(z)
        log_1m = jnp.where(causal, jax.nn.log_sigmoid(-z), 0.0)
        after = lax.cumsum(log_1m, axis=3, reverse=True) - log_1m
        a = jnp.where(causal, jnp.exp(log_beta + after), 0.0)
        outs.append(jnp.einsum('bhts,bhsd->bhtd', a, v[:, :, :t1]))
    return jnp.concatenate(outs, axis=2)


def hybrid_mixer(h, w_in, conv_w, a_log, dt_bias, dn_out_gain, sb_q_gain, sb_k_gain, sb_out_gain, w_out):
    b, s, _ = h.shape
    f32 = jnp.float32
    proj = h @ w_in
    offs = [int(o) for o in np.cumsum(IN_SIZES)[:-1]]
    dq, dk, dv, dz, db, da, sq, sk, sv = jnp.split(proj, offs, axis=-1)
    qkv = jax.nn.silu(causal_depthwise_conv(jnp.concatenate([dq, dk, dv], axis=-1), conv_w))
    dq, dk, dv = jnp.split(qkv, 3, axis=-1)
    q = l2_norm(dq.reshape(b, s, DN_HEADS, DN_HEAD_DIM)) * DN_HEAD_DIM ** -0.5
    k = l2_norm(dk.reshape(b, s, DN_HEADS, DN_HEAD_DIM))
    v = dv.reshape(b, s, DN_HEADS, DN_HEAD_DIM).astype(f32)
    beta = jax.nn.sigmoid(db.astype(f32))
    g = -jnp.exp(a_log.astype(f32)) * jax.nn.softplus(da.astype(f32) + dt_bias.astype(f32))
    o = gated_delta_rule(q.transpose(0, 2, 1, 3), k.transpose(0, 2, 1, 3), v.transpose(0, 2, 1, 3),
                         g.transpose(0, 2, 1), beta.transpose(0, 2, 1)).transpose(0, 2, 1, 3)
    o_dn = rms_norm(o, dn_out_gain) * jax.nn.silu(dz.reshape(b, s, DN_HEADS, DN_HEAD_DIM).astype(f32))
    o_dn = o_dn.reshape(b, s, DN_WIDTH)
    qs = rms_norm(sq.reshape(b, s, SB_HEADS, SB_HEAD_DIM), sb_q_gain).astype(f32).transpose(0, 2, 1, 3)
    ks = rms_norm(sk.reshape(b, s, SB_HEADS, SB_HEAD_DIM), sb_k_gain).astype(f32).transpose(0, 2, 1, 3)
    vs = sv.reshape(b, s, SB_HEADS, SB_HEAD_DIM).astype(f32).transpose(0, 2, 1, 3)
    o_sb = stick_breaking_attention(qs, ks, vs).transpose(0, 2, 1, 3)
    o_sb = rms_norm(o_sb, sb_out_gain).reshape(b, s, SB_WIDTH)
    mixed = jnp.concatenate([o_dn, o_sb], axis=-1).astype(h.dtype)
    return mixed @ w_out


def peer_ffn(h, w_query, sub_keys, w_u, w_v):
    b, s, d = h.shape
    hb = h.reshape(b * s // PEER_TOKEN_BLOCK, PEER_TOKEN_BLOCK, d)

    def block(ht):
        t = ht.shape[0]
        q = (ht @ w_query).reshape(t, PEER_HEADS, 2, PEER_HALF)
        scores = jnp.einsum('thpk,hpnk->thpn', q, sub_keys)
        sv, si = lax.top_k(scores, PEER_TOPK)
        cand_s = sv[:, :, 0, :, None] + sv[:, :, 1, None, :]
        cand_i = si[:, :, 0, :, None] * PEER_N_KEYS + si[:, :, 1, None, :]
        top_s, top_j = lax.top_k(cand_s.reshape(t, PEER_HEADS, PEER_TOPK * PEER_TOPK), PEER_TOPK)
        expert = jnp.take_along_axis(cand_i.reshape(t, PEER_HEADS, PEER_TOPK * PEER_TOPK), top_j, axis=-1)
        gate = jax.nn.softmax(top_s.astype(jnp.float32), axis=-1)
        pre = jnp.einsum('td,thkd->thk', ht, w_u[expert]).astype(jnp.float32)
        act = jax.nn.gelu(pre, approximate=False) * gate
        return jnp.einsum('thk,thkd->td', act.astype(ht.dtype), w_v[expert])

    return lax.map(block, hb).reshape(b, s, d)


def setup_inputs(seed: int = 0) -> dict:
    key = jax.random.key(seed)
    ks = jax.random.split(key, 20)
    f32 = jnp.float32
    d = D_MODEL
    dt = jnp.exp(jax.random.uniform(ks[6], (DEPTH, DN_HEADS), f32, np.log(1e-3), np.log(1e-1)))
    return {
        "x": jax.random.normal(ks[0], (BATCH, SEQ, d), f32),
        "c": jax.random.normal(ks[1], (BATCH, d), f32),
        "w_ada": jax.random.normal(ks[2], (DEPTH, d, N_MOD * d), f32) * (0.5 * d ** -0.5),
        "b_ada": jax.random.normal(ks[3], (DEPTH, N_MOD * d), f32) * 0.02,
        "norm1_gain": 1.0 + 0.02 * jax.random.normal(ks[4], (DEPTH, d), f32),
        "w_in": jax.random.normal(ks[5], (DEPTH, d, IN_COLS), f32) * d ** -0.5,
        "dn_conv_w": jax.random.normal(ks[7], (DEPTH, DN_CONV, 3 * DN_WIDTH), f32) * DN_CONV ** -0.5,
        "dn_a_log": jnp.log(jax.random.uniform(ks[8], (DEPTH, DN_HEADS), f32, 1.0, 16.0)),
        "dn_dt_bias": dt + jnp.log(-jnp.expm1(-dt)),
        "dn_out_gain": 1.0 + 0.02 * jax.random.normal(ks[9], (DEPTH, DN_HEAD_DIM), f32),
        "sb_q_gain": 1.0 + 0.02 * jax.random.normal(ks[10], (DEPTH, SB_HEAD_DIM), f32),
        "sb_k_gain": 1.0 + 0.02 * jax.random.normal(ks[11], (DEPTH, SB_HEAD_DIM), f32),
        "sb_out_gain": 1.0 + 0.02 * jax.random.normal(ks[12], (DEPTH, SB_HEAD_DIM), f32),
        "w_out": jax.random.normal(ks[13], (DEPTH, MIX_WIDTH, d), f32) * MIX_WIDTH ** -0.5,
        "norm2_gain": 1.0 + 0.02 * jax.random.normal(ks[14], (DEPTH, d), f32),
        "peer_w_query": jax.random.normal(ks[15], (DEPTH, d, PEER_HEADS * PEER_QUERY_DIM), f32) * d ** -0.5,
        "peer_sub_keys": jax.random.normal(ks[16], (DEPTH, PEER_HEADS, 2, PEER_N_KEYS, PEER_HALF), f32) * PEER_HALF ** -0.5,
        "peer_w_u": jax.random.normal(ks[17], (DEPTH, PEER_EXPERTS, d), f32) * d ** -0.5,
        "peer_w_v": jax.random.normal(ks[18], (DEPTH, PEER_EXPERTS, d), f32),
    }


def reference(x, c, w_ada, b_ada, norm1_gain, w_in, dn_conv_w, dn_a_log, dn_dt_bias, dn_out_gain,
              sb_q_gain, sb_k_gain, sb_out_gain, w_out, norm2_gain, peer_w_query, peer_sub_keys,
              peer_w_u, peer_w_v):
    for l in range(DEPTH):
        mod = jax.nn.silu(c) @ w_ada[l] + b_ada[l]
        shift1, scale1, gate1, shift2, scale2, gate2 = jnp.split(mod[:, None, :], N_MOD, axis=-1)
        h = rms_norm(x, norm1_gain[l]) * (1.0 + scale1) + shift1
        mixed = hybrid_mixer(h, w_in[l], dn_conv_w[l], dn_a_log[l], dn_dt_bias[l], dn_out_gain[l],
                             sb_q_gain[l], sb_k_gain[l], sb_out_gain[l], w_out[l])
        x = x + gate1 * mixed
        h = rms_norm(x, norm2_gain[l]) * (1.0 + scale2) + shift2
        x = x + gate2 * peer_ffn(h, peer_w_query[l], peer_sub_keys[l], peer_w_u[l], peer_w_v[l])
    return x
```

```python
import numpy as np
import concourse.bass as bass
import concourse.mybir as mybir
from concourse.bass_utils import run_bass_kernel_spmd
from contextlib import ExitStack

F32 = mybir.dt.float32
BF16 = mybir.dt.bfloat16
U32 = mybir.dt.uint32
I32 = mybir.dt.int32
AF = mybir.ActivationFunctionType
ALU = mybir.AluOpType
AX = mybir.AxisListType

N_DMA_SEMS = 12
SAME_ENG_SYNC = True


class Buf:
    __slots__ = ("name", "last_w", "reads")

    def __init__(self, name):
        self.name = name
        self.last_w = []
        self.reads = []


class KB:
    def __init__(self, nc, es):
        self.nc = nc
        self.es = es
        self.engs = {"pe": nc.tensor, "act": nc.scalar, "dve": nc.vector, "pool": nc.gpsimd, "sp": nc.sync}
        self.q = {e: [] for e in self.engs}
        self.cnt = {}
        self.sems = {}
        self.waited = {e: {} for e in self.engs}
        for e in self.engs:
            self.sems[e] = es.enter_context(nc.semaphore("s_" + e))
            self.cnt[e] = 0
        self.dma_rr = {}
        for e in ("sp", "pool", "act"):
            for i in range(N_DMA_SEMS):
                k = "d_%s_%d" % (e, i)
                self.sems[k] = es.enter_context(nc.semaphore(k))
                self.cnt[k] = 0
            self.dma_rr[e] = 0
        self.nbuf = 0

    def sb(self, name, shape, dt):
        t = self.es.enter_context(self.nc.sbuf_tensor(name, list(shape), dt))
        return t

    def buf(self, name=None):
        self.nbuf += 1
        return Buf(name or ("b%d" % self.nbuf))

    def _waits(self, eng, reads, writes):
        deps = []
        for b in reads:
            deps += b.last_w
        for b in writes:
            deps += b.last_w
            deps += b.reads
        need = {}
        for (k, v) in deps:
            if k == eng and (eng == "pe" or eng == "sp" or (not SAME_ENG_SYNC and eng != "pool")):
                continue
            if need.get(k, 0) < v:
                need[k] = v
        out = []
        w = self.waited[eng]
        for k, v in need.items():
            if w.get(k, 0) >= v:
                continue
            w[k] = v
            out.append((k, v))
        return out

    def _commit(self, ev, reads, writes):
        for b in writes:
            b.last_w = [ev]
            b.reads = []
        for b in reads:
            if b not in writes:
                b.reads.append(ev)
                if len(b.reads) > 64:
                    mx = {}
                    for (k, v) in b.reads:
                        if mx.get(k, 0) < v:
                            mx[k] = v
                    b.reads = list(mx.items())

    def op(self, eng, fn, reads=(), writes=()):
        reads = list(reads)
        writes = list(writes)
        waits = self._waits(eng, reads, writes)
        self.cnt[eng] += 1
        ev = (eng, self.cnt[eng])
        self._commit(ev, reads, writes)
        self.q[eng].append((waits, fn, eng, 1))
        return ev

    def dma(self, eng, out, in_, reads=(), writes=(), **kw):
        reads = list(reads)
        writes = list(writes)
        waits = self._waits(eng, reads, writes)
        i = self.dma_rr[eng]
        self.dma_rr[eng] = (i + 1) % N_DMA_SEMS
        k = "d_%s_%d" % (eng, i)
        if self.cnt[k] > 0 and self.waited[eng].get(k, 0) < self.cnt[k]:
            self.waited[eng][k] = self.cnt[k]
            waits.append((k, self.cnt[k]))
        self.cnt[k] += 16
        ev = (k, self.cnt[k])
        self._commit(ev, reads, writes)
        self.q[eng].append((waits, (lambda e, o=out, i_=in_, kw=kw: e.dma_start(out=o, in_=i_, **kw)), k, 16))
        return ev

    def barrier(self):
        snap = dict(self.cnt)
        for eng in self.engs:
            waits = []
            w = self.waited[eng]
            for k, v in snap.items():
                if v == 0 or k == eng:
                    continue
                if w.get(k, 0) >= v:
                    continue
                w[k] = v
                waits.append((k, v))
            if waits:
                self.q[eng].append((waits, None, None, 0))

    def final_wait(self, eng="sp"):
        snap = dict(self.cnt)
        waits = [(k, v) for k, v in snap.items() if v > 0 and k != eng]
        self.q[eng].append((waits, None, None, 0))

    def emit(self):
        nc = self.nc
        sems = self.sems

        def replay(name, e):
            for (waits, fn, inck, incv) in self.q[name]:
                for (k, v) in waits:
                    e.wait_ge(sems[k], v)
                if fn is not None:
                    ins = fn(e)
                    ins.then_inc(sems[inck], incv)

        with nc.Block() as block:
            @block.tensor
            def _(e):
                replay("pe", e)

            @block.scalar
            def _(e):
                replay("act", e)

            @block.vector
            def _(e):
                replay("dve", e)

            @block.gpsimd
            def _(e):
                replay("pool", e)

            @block.sync
            def _(e):
                replay("sp", e)


import os
SBSTAGE = int(os.environ.get('SBSTAGE', '9'))
PREP = int(os.environ.get('PREP', '9'))
DNSTAGE = int(os.environ.get('DNSTAGE', '9'))

S = 4096
D = 1024
NT = 32
INC = 3592
EPS = 1e-6


class Tl:
    __slots__ = ("t", "b")

    def __init__(self, t, b):
        self.t = t
        self.b = b


def bl(ts):
    return [t.b for t in ts]


class Ctx:
    pass


def mk(kb):
    h = Ctx()

    def tile(name, shape, dt, es=None):
        t = (es or kb.es).enter_context(kb.nc.sbuf_tensor(name, list(shape), dt))
        return Tl(t, kb.buf(name))

    def ptile(name, shape, dt, es=None):
        t = (es or kb.es).enter_context(kb.nc.psum_tensor(name, list(shape), dt))
        return Tl(t, kb.buf(name))

    def ACT(out, in_, func, r, w, **kw):
        kb.op("act", lambda e: e.activation(out=out, in_=in_, func=func, **kw), bl(r), bl(w))

    def TT(eng, out, in0, in1, op, r, w):
        kb.op(eng, lambda e: e.tensor_tensor(out=out, in0=in0, in1=in1, op=op), bl(r), bl(w))

    def TS(eng, out, in0, s1, s2, op0, op1, r, w):
        if op1 is None:
            kb.op(eng, lambda e: e.tensor_scalar(out, in0, s1, None, op0), bl(r), bl(w))
        else:
            kb.op(eng, lambda e: e.tensor_scalar(out, in0, s1, s2, op0, op1), bl(r), bl(w))

    def STT(eng, out, in0, scalar, in1, op0, op1, r, w):
        kb.op(eng, lambda e: e.scalar_tensor_tensor(out=out, in0=in0, scalar=scalar, in1=in1, op0=op0, op1=op1), bl(r), bl(w))

    def CP(eng, out, in_, r, w):
        if eng == "act":
            kb.op("act", lambda e: e.activation(out=out, in_=in_, func=AF.Copy), bl(r), bl(w))
        else:
            kb.op(eng, lambda e: e.tensor_copy(out, in_), bl(r), bl(w))

    def MM(out, lhsT, rhs, start, stop, r, w):
        kb.op("pe", lambda e: e.matmul(out, lhsT=lhsT, rhs=rhs, start=start, stop=stop), bl(r), bl(w))

    def TR(out, in_, ident, r, w):
        kb.op("pe", lambda e: e.transpose(out, in_, ident), bl(r), bl(w))

    def DMA(eng, out, in_, r, w, **kw):
        kb.dma(eng, out, in_, bl(r), bl(w), **kw)

    def MEMSET(eng, ap, val, w):
        kb.op(eng, lambda e: e.memset(ap, val), [], bl(w))

    def ASEL(out, in_, pattern, base, cm, cmp, fill, r, w):
        kb.op("pool", lambda e: e.affine_select(out=out, in_=in_, pattern=pattern, base=base, channel_multiplier=cm,
                                                 compare_op=cmp, fill=fill), bl(r), bl(w))

    def RECIP(out, in_, r, w):
        kb.op("dve", lambda e: e.reciprocal(out, in_), bl(r), bl(w))

    for k, v in list(locals().items()):
        if k not in ("h", "kb"):
            setattr(h, k, v)
    return h


def build(dbg=(), nt=NT, upto=9, skip=()):
    nc = bass.Bass("TRN2", target_bir_lowering=False)
    dbg = set(dbg)

    def din(name, shape):
        return nc.dram_tensor(name, list(shape), F32, kind="ExternalInput").ap()

    def scratch(name, shape, dt):
        kind = "ExternalOutput" if name in dbg else "Internal"
        return Tl(nc.dram_tensor(name, list(shape), dt, kind=kind).ap(), Buf(name))

    x = din("x", [S, D])
    c = din("c", [1, D])
    w_ada = din("w_ada", [D, 6 * D])
    b_ada = din("b_ada", [1, 6 * D])
    norm1_gain = din("norm1_gain", [1, D])
    w_in = din("w_in", [D, INC])
    dn_conv_w = din("dn_conv_w", [1, 6144])
    dn_a_log = din("dn_a_log", [1, 4])
    dn_dt_bias = din("dn_dt_bias", [1, 4])
    dn_out_gain = din("dn_out_gain", [1, 128])
    sb_q_gain = din("sb_q_gain", [1, 64])
    sb_k_gain = din("sb_k_gain", [1, 64])
    sb_out_gain = din("sb_out_gain", [1, 64])
    w_out = din("w_out", [D, D])
    norm2_gain = din("norm2_gain", [1, D])
    peer_w_query = din("peer_w_query", [D, 2048])
    peer_sub_keys = din("peer_sub_keys", [16, 128, 128])
    peer_w_u = din("peer_w_u", [16384, D])
    peer_w_v = din("peer_w_v", [16384, D])
    out = nc.dram_tensor("out", [S, D], F32, kind="ExternalOutput").ap()
    OUT = Tl(out, Buf("out"))

    proj = scratch("proj", [S, INC], F32)
    mixed = scratch("mixed", [S, D], BF16)
    x1 = scratch("x1", [S, D], F32)
    moddbg = scratch("moddbg", [128, 6, D], F32) if "moddbg" in dbg else None
    NCH = int(os.environ.get("NCH", "128"))
    h2T_d = scratch("h2T_d", [128, 8, S], BF16)
    sc_d = scratch("sc_d", [S, 16, 128], F32)
    qT_d = scratch("qT_d", [4, 128, S], BF16)
    kT_d = scratch("kT_d", [4, 128, S], BF16)
    v_d = scratch("v_d", [S, 512], BF16)
    GT_d = scratch("GT_d", [128, 128, S], BF16)
    wuT_d = scratch("wuT_d", [128, 128, 8, 128], BF16)
    wv_d = scratch("wv_d", [128, 128, D], BF16)

    with ExitStack() as ges:
        kb = KB(nc, ges)
        h = mk(kb)
        G = Ctx()
        G.identf = h.tile("identf", [128, 128], F32)
        G.ident = h.tile("ident", [128, 128], BF16)
        G.onesf = h.tile("onesf", [128, 128], F32)
        G.m_li = h.tile("m_li", [128, 128], F32)
        G.m_ls = h.tile("m_ls", [128, 128], F32)
        G.m_ui = h.tile("m_ui", [128, 128], F32)
        G.m_us = h.tile("m_us", [128, 128], F32)
        G.mod = [h.tile("mod%d" % i, [128, D], F32) for i in range(6)]
        h.MEMSET("pool", G.identf.t[:], 0.0, [G.identf])
        h.ASEL(G.identf.t[:], G.identf.t[:], [[-1, 128]], 0, 1, ALU.not_equal, 1.0, [G.identf], [G.identf])
        h.CP("dve", G.ident.t[:], G.identf.t[:], [G.identf], [G.ident])
        h.MEMSET("pool", G.onesf.t[:], 1.0, [G.onesf])
        h.ASEL(G.m_li.t[:], G.onesf.t[:], [[-1, 128]], 0, 1, ALU.is_ge, 0.0, [G.onesf], [G.m_li])
        h.ASEL(G.m_ls.t[:], G.onesf.t[:], [[-1, 128]], 0, 1, ALU.is_gt, 0.0, [G.onesf], [G.m_ls])
        h.ASEL(G.m_ui.t[:], G.onesf.t[:], [[1, 128]], 0, -1, ALU.is_ge, 0.0, [G.onesf], [G.m_ui])
        h.ASEL(G.m_us.t[:], G.onesf.t[:], [[1, 128]], 0, -1, ALU.is_gt, 0.0, [G.onesf], [G.m_us])

        with ExitStack() as pes:
            kb.es = pes
            c_sb = h.tile("c_sb", [128, 8], F32)
            crep = h.tile("crep", [128, 8, 128], F32)
            brow = h.tile("brow", [1, 6 * D], F32)
            wa = [h.tile("wa%d" % i, [128, 8, 512], F32) for i in range(2)]
            ng = [h.tile("ng%d" % i, [128, D], F32) for i in range(2)]
            pm = [h.ptile("pm%d" % i, [128, 512], F32) for i in range(2)]
            h.DMA("sp", c_sb.t[:], c.rearrange("o (k p) -> p (o k)", p=128), [], [c_sb], allow_slow_non_contiguous=True)
            h.DMA("sp", brow.t[:], b_ada, [], [brow])
            h.DMA("sp", ng[0].t[:], norm1_gain.to_broadcast([128, D]), [], [ng[0]])
            h.DMA("sp", ng[1].t[:], norm2_gain.to_broadcast([128, D]), [], [ng[1]])
            h.ACT(c_sb.t[:], c_sb.t[:], AF.Silu, [c_sb], [c_sb])
            h.CP("dve", crep.t[:], c_sb.t[:].unsqueeze(2).to_broadcast([128, 8, 128]), [c_sb], [crep])
            wav = w_ada.rearrange("(k p) n -> p k n", p=128)
            for cg in range(12):
                w_ = wa[cg % 2]
                p_ = pm[cg % 2]
                h.DMA("sp", w_.t[:], wav[:, :, cg * 512:(cg + 1) * 512], [], [w_])
                for k in range(8):
                    h.MM(p_.t[:], crep.t[:, k, :], w_.t[:, k, :], k == 0, False, [crep, w_], [p_])
                h.MM(p_.t[:], G.onesf.t[0:1, :], brow.t[0:1, cg * 512:(cg + 1) * 512], False, True, [G.onesf, brow], [p_])
                m_ = G.mod[cg // 2]
                h.CP("act" if cg % 2 else "dve", m_.t[:, (cg % 2) * 512:(cg % 2 + 1) * 512], p_.t[:], [p_], [m_])
            if moddbg is not None:
                for i in range(6):
                    h.DMA("sp", moddbg.t[:, i, :], G.mod[i].t[:], [G.mod[i]], [moddbg])
            h.STT("dve", G.mod[1].t[:], G.mod[1].t[:], 1.0, ng[0].t[:], ALU.add, ALU.mult, [G.mod[1], ng[0]], [G.mod[1]])
            h.STT("dve", G.mod[4].t[:], G.mod[4].t[:], 1.0, ng[1].t[:], ALU.add, ALU.mult, [G.mod[4], ng[1]], [G.mod[4]])
            kb.barrier()
        G.S1, G.G1, G.gate1, G.S2, G.G2, G.gate2 = G.mod

        def run_window(gen_fns, width):
            it = iter(gen_fns)
            active = []
            while True:
                while len(active) < width:
                    f = next(it, None)
                    if f is None:
                        break
                    active.append(f())
                if not active:
                    break
                for g_ in list(active):
                    try:
                        next(g_)
                    except StopIteration:
                        active.remove(g_)

        def norm_mod_T(xt, Gt, St, junk, hb, ss, rstd, pT, hT):
            h.ACT(junk.t[:], xt.t[:], AF.Square, [xt], [junk, ss], accum_out=ss.t[:])
            h.ACT(rstd.t[:], ss.t[:], AF.Ln, [ss], [rstd], scale=1.0 / D, bias=EPS)
            h.ACT(rstd.t[:], rstd.t[:], AF.Exp, [rstd], [rstd], scale=-0.5)
            h.STT("dve", junk.t[:], xt.t[:], rstd.t[:], Gt.t[:], ALU.mult, ALU.mult, [xt, rstd, Gt], [junk])
            h.TT("pool", hb.t[:], junk.t[:], St.t[:], ALU.add, [junk, St], [hb])
            for k in range(8):
                h.TR(pT.t[:, k, :], hb.t[:, k * 128:(k + 1) * 128], G.ident.t[:], [hb, G.ident], [pT])
            h.CP("act", hT.t[:], pT.t[:], [pT], [hT])

        if upto >= 1:
            with ExitStack() as pes:
                kb.es = pes
                winb = h.tile("winb", [128, 8, INC], BF16)
                wst = [h.tile("wst%d" % i, [128, 8, 512], F32) for i in range(2)]
                xt = [h.tile("xt%d" % i, [128, D], F32) for i in range(2)]
                junk_l = [h.tile("junk%d" % i, [128, D], F32) for i in range(2)]
                hb_l = [h.tile("hb%d" % i, [128, D], BF16) for i in range(2)]
                ss_l = [h.tile("ss%d" % i, [128, 1], F32) for i in range(2)]
                rstd_l = [h.tile("rstd%d" % i, [128, 1], F32) for i in range(2)]
                hT = [h.tile("hT%d" % i, [128, 8, 128], BF16) for i in range(2)]
                yt = [h.tile("yt%d" % i, [128, INC], F32) for i in range(2)]
                pT = h.ptile("pT", [128, 8, 128], BF16)
                py = [h.ptile("py%d" % i, [128, 512], F32) for i in range(3)]
                wiv = w_in.rearrange("(k p) n -> p k n", p=128)
                for cg in range(8):
                    c0 = cg * 512
                    w = min(512, INC - c0)
                    h.DMA("sp", wst[cg % 2].t[:, :, 0:w], wiv[:, :, c0:c0 + w], [], [wst[cg % 2]])
                    h.CP("dve" if cg % 2 else "pool", winb.t[:, :, c0:c0 + w], wst[cg % 2].t[:, :, 0:w], [wst[cg % 2]], [winb])
                def p1_tile(tt):
                    X = xt[tt % 2]
                    h.DMA("sp", X.t[:], x[tt * 128:(tt + 1) * 128, :], [], [X])
                    HT = hT[tt % 2]
                    norm_mod_T(X, G.G1, G.S1, junk_l[tt % 2], hb_l[tt % 2], ss_l[tt % 2], rstd_l[tt % 2], pT, HT)
                    yield
                    Y = yt[tt % 2]
                    for cg in range(8):
                        c0 = cg * 512
                        w = min(512, INC - c0)
                        P = py[cg % 3]
                        for k in range(8):
                            h.MM(P.t[:, 0:w], HT.t[:, k, :], winb.t[:, k, c0:c0 + w], k == 0, k == 7, [HT, winb], [P])
                        h.CP("act" if cg % 2 else "dve", Y.t[:, c0:c0 + w], P.t[:, 0:w], [P], [Y])
                        if cg % 2 == 1:
                            yield
                    h.DMA("sp", proj.t[tt * 128:(tt + 1) * 128, :], Y.t[:], [Y], [proj])
                run_window([(lambda tt=tt: p1_tile(tt)) for tt in range(nt)], 2)
                kb.barrier()


        if upto >= 2 and 2 not in skip:
            with ExitStack() as pes:
                kb.es = pes
                cw = h.tile("cw", [128, 4, 1536], F32)
                dtb = h.tile("dtb", [128, 4], F32)
                nA = h.tile("nA", [128, 4], F32)
                dgain = h.tile("dgain", [128, 128], F32)
                sel4 = h.tile("sel4", [4, 4, 128], F32)
                BD = [h.tile("BD%d" % k, [128, 128], F32) for k in range(2)]
                DB = [h.tile("DB%d" % k, [128, 128], F32) for k in range(7)]
                h.DMA("sp", cw.t[:].rearrange("p k c -> p (k c)"), dn_conv_w.to_broadcast([128, 6144]), [], [cw])
                h.DMA("sp", dtb.t[:], dn_dt_bias.to_broadcast([128, 4]), [], [dtb])
                h.DMA("sp", nA.t[:], dn_a_log.to_broadcast([128, 4]), [], [nA])
                h.DMA("sp", dgain.t[:], dn_out_gain.to_broadcast([128, 128]), [], [dgain])
                h.ACT(nA.t[:], nA.t[:], AF.Exp, [nA], [nA])
                h.TS("dve", nA.t[:], nA.t[:], -1.0, None, ALU.mult, None, [nA], [nA])
                for hh in range(4):
                    h.CP("dve", sel4.t[:, hh, :], G.identf.t[0:4, hh:hh + 1].to_broadcast([4, 128]), [G.identf], [sel4])
                h.CP("dve", BD[0].t[:], G.identf.t[:], [G.identf], [BD[0]])
                for k in range(7):
                    cur = BD[k % 2]
                    nxt = BD[(k + 1) % 2]
                    b = 2 ** (k + 1)
                    if b == 128:
                        h.CP("dve", nxt.t[:], G.onesf.t[:], [G.onesf], [nxt])
                    else:
                        nb = 128 // b
                        h.ASEL(nxt.t[:].rearrange("p (a b) -> p a b", b=b), G.onesf.t[:].rearrange("p (a b) -> p a b", b=b),
                               [[-b, nb], [0, b]], 0, 1, ALU.is_ge, 0.0, [G.onesf], [nxt])
                        h.ASEL(nxt.t[:].rearrange("p (a b) -> p a b", b=b), nxt.t[:].rearrange("p (a b) -> p a b", b=b),
                               [[b, nb], [0, b]], b - 1, -1, ALU.is_ge, 0.0, [nxt], [nxt])
                    h.TT("dve", DB[k].t[:], nxt.t[:], cur.t[:], ALU.subtract, [nxt, cur], [DB[k]])
                u4 = [[h.tile("u4_%d_%d" % (i, k), [128, 1536], F32) for k in range(4)] for i in range(2)]
                zin = [h.tile("zin%d" % i, [128, 520], F32) for i in range(2)]
                acc = h.tile("acc", [128, 1536], F32)
                tmpc = [h.tile("tmpc%d" % i, [128, 1536], F32) for i in range(2)]
                C2 = []
                for i in range(2):
                    c_ = Ctx()
                    c_.qkv = h.tile("qkv%d" % i, [128, 1536], F32)
                    c_.ssum = h.tile("dssum%d" % i, [128, 8], F32)
                    c_.dgz = h.tile("dgz%d" % i, [128, 512], F32)
                    c_.beta = h.tile("beta%d" % i, [128, 4], F32)
                    c_.gg = h.tile("gg%d" % i, [128, 4], F32)
                    c_.gcs = h.tile("gcs%d" % i, [128, 8], F32)
                    c_.eg = h.tile("eg%d" % i, [128, 4], F32)
                    c_.ekd = h.tile("ekd%d" % i, [128, 4], F32)
                    c_.egl = h.tile("egl%d" % i, [128, 4], F32)
                    c_.be = h.tile("be%d" % i, [128, 4], F32)
                    c_.gcT = h.tile("gcT%d" % i, [4, 128], F32)
                    c_.odn = h.tile("odn%d" % i, [128, 512], BF16)
                    C2.append(c_)
                Sf = [h.tile("Sf%d" % i, [128, 128], F32) for i in range(4)]
                Sb = [h.tile("Sb%d" % i, [128, 128], BF16) for i in range(4)]
                SL = []
                for i in range(2):
                    s_ = Ctx()
                    for nm in ("dec", "decT", "dtmp", "qn", "kn", "qd", "Lf", "us", "ojunk", "on"):
                        setattr(s_, nm, h.tile("%s_%d" % (nm, i), [128, 128], F32))
                    for nm in ("qT", "kT", "qdT", "LT", "attnT", "EkT", "Xs", "Dm", "DmT", "vb", "kbg", "kdec", "wT", "vnew"):
                        setattr(s_, nm, h.tile("%s_%d" % (nm, i), [128, 128], BF16))
                    s_.oss = h.tile("doss_%d" % i, [128, 1], F32)
                    s_.bT = h.ptile("bT_%d" % i, [128, 4, 128], F32)
                    s_.bA = h.ptile("bA_%d" % i, [128, 4, 128], F32)
                    s_.bB = h.ptile("bB_%d" % i, [128, 4, 128], F32)
                    s_.bK = h.ptile("bK_%d" % i, [128, 4, 128], F32)
                    SL.append(s_)
                pgb = SL[0].bK
                for hh in range(4):
                    h.MEMSET("pool", Sf[hh].t[:], 0.0, [Sf[hh]])
                    h.MEMSET("pool", Sb[hh].t[:], 0.0, [Sb[hh]])

                def prologue(n):
                    r0 = n * 128
                    U = u4[n % 2]
                    Z = zin[n % 2]
                    c_ = C2[n % 2]
                    qkv = c_.qkv; ssum = c_.ssum; beta = c_.beta; gg = c_.gg; gcs = c_.gcs
                    for k in range(4):
                        sh = 3 - k
                        if n == 0 and sh > 0:
                            h.MEMSET("pool", U[k].t[:], 0.0, [U[k]])
                            h.DMA("sp", U[k].t[sh:128, :], proj.t[0:128 - sh, 0:1536], [proj], [U[k]])
                        else:
                            h.DMA("sp", U[k].t[:], proj.t[r0 - sh:r0 - sh + 128, 0:1536], [proj], [U[k]])
                    h.DMA("sp", Z.t[:], proj.t[r0:r0 + 128, 1536:2056], [proj], [Z])
                    h.TT("dve", acc.t[:], U[3].t[:], cw.t[:, 3, :], ALU.mult, [U[3], cw], [acc])
                    yield
                    for k in range(3):
                        T_ = tmpc[k % 2]
                        h.TT("pool", T_.t[:], U[k].t[:], cw.t[:, k, :], ALU.mult, [U[k], cw], [T_])
                        h.TT("dve", acc.t[:], acc.t[:], T_.t[:], ALU.add, [acc, T_], [acc])
                        yield
                    h.ACT(qkv.t[:], acc.t[:], AF.Silu, [acc], [qkv])
                    h.ACT(c_.dgz.t[:], Z.t[:, 0:512], AF.Silu, [Z], [c_.dgz])
                    h.ACT(beta.t[:], Z.t[:, 512:516], AF.Sigmoid, [Z], [beta])
                    h.TT("pool", c_.dgz.t[:].rearrange("p (a b) -> p a b", b=128), c_.dgz.t[:].rearrange("p (a b) -> p a b", b=128),
                         dgain.t[:].unsqueeze(1).to_broadcast([128, 4, 128]), ALU.mult, [c_.dgz, dgain], [c_.dgz])
                    yield
                    h.TT("dve", acc.t[:, 0:1024], qkv.t[:, 0:1024], qkv.t[:, 0:1024], ALU.mult, [qkv], [acc])
                    kb.op("dve", lambda e, ssum=ssum, acc=acc: e.tensor_reduce(out=ssum.t[:], in_=acc.t[:, 0:1024].rearrange("p (a b) -> p a b", b=128),
                                                                           axis=AX.X, op=ALU.add), bl([acc]), bl([ssum]))
                    h.ACT(ssum.t[:], ssum.t[:], AF.Ln, [ssum], [ssum], bias=EPS)
                    h.ACT(ssum.t[:], ssum.t[:], AF.Exp, [ssum], [ssum], scale=-0.5)
                    h.TS("dve", ssum.t[:, 0:4], ssum.t[:, 0:4], 128.0 ** -0.5, None, ALU.mult, None, [ssum], [ssum])
                    yield
                    h.TT("dve", gg.t[:], Z.t[:, 516:520], dtb.t[:], ALU.add, [Z, dtb], [gg])
                    h.ACT(gg.t[:], gg.t[:], AF.Exp, [gg], [gg])
                    h.ACT(gg.t[:], gg.t[:], AF.Ln, [gg], [gg], bias=1.0)
                    h.TT("dve", gg.t[:], gg.t[:], nA.t[:], ALU.mult, [gg, nA], [gg])
                    h.MM(pgb.t[:, 3, 0:4], G.m_ui.t[:], gg.t[:], True, True, [G.m_ui, gg], [pgb])
                    h.MM(pgb.t[:, 3, 4:8], G.onesf.t[:], gg.t[:], True, True, [G.onesf, gg], [pgb])
                    h.CP("dve", gcs.t[:], pgb.t[:, 3, 0:8], [pgb], [gcs])
                    yield
                    h.ACT(c_.eg.t[:], gcs.t[:, 0:4], AF.Exp, [gcs], [c_.eg])
                    h.ACT(c_.egl.t[:], gcs.t[:, 4:8], AF.Exp, [gcs], [c_.egl])
                    h.TT("dve", c_.ekd.t[:], gcs.t[:, 4:8], gcs.t[:, 0:4], ALU.subtract, [gcs], [c_.ekd])
                    h.ACT(c_.ekd.t[:], c_.ekd.t[:], AF.Exp, [c_.ekd], [c_.ekd])
                    h.TT("dve", c_.be.t[:], beta.t[:], c_.eg.t[:], ALU.mult, [beta, c_.eg], [c_.be])
                    h.TR(pgb.t[0:4, 3, 0:128], gcs.t[:, 0:4], G.identf.t[:], [gcs, G.identf], [pgb])
                    h.CP("dve", c_.gcT.t[:], pgb.t[0:4, 3, 0:128], [pgb], [c_.gcT])
                    yield

                def head(n, hh, sl):
                    c_ = C2[n % 2]
                    s_ = SL[sl]
                    qkv = c_.qkv; ssum = c_.ssum; beta = c_.beta; gcs = c_.gcs
                    qs = qkv.t[:, hh * 128:(hh + 1) * 128]
                    ks = qkv.t[:, 512 + hh * 128:512 + (hh + 1) * 128]
                    vs = qkv.t[:, 1024 + hh * 128:1024 + (hh + 1) * 128]
                    gch = gcs.t[:, hh:hh + 1]
                    bT, bA, bB, bK = s_.bT, s_.bA, s_.bB, s_.bK
                    pX = Tl(bA.t[:, 0, :], bA.b); pU = Tl(bA.t[:, 1, :], bA.b); pW = Tl(bA.t[:, 2, :], bA.b); pO = Tl(bA.t[:, 3, :], bA.b)
                    pY = Tl(bB.t[:, 0, :], bB.b); pYT = Tl(bB.t[:, 1, :], bB.b); pS1 = Tl(bB.t[:, 2, :], bB.b); pSn = Tl(bB.t[:, 3, :], bB.b)
                    pR = Tl(bK.t[:, 0, :], bK.b); pk = Tl(bK.t[:, 1, :], bK.b); pKQ = Tl(bK.t[:, 2, :], bK.b)
                    ptr = [Tl(bT.t[:, i, :], bT.b) for i in range(4)]
                    h.TS("dve", s_.qn.t[:], qs, ssum.t[:, hh:hh + 1], None, ALU.mult, None, [qkv, ssum], [s_.qn])
                    h.TS("dve", s_.kn.t[:], ks, ssum.t[:, 4 + hh:5 + hh], None, ALU.mult, None, [qkv, ssum], [s_.kn])
                    h.TS("dve", s_.qd.t[:], s_.qn.t[:], c_.eg.t[:, hh:hh + 1], None, ALU.mult, None, [s_.qn, c_.eg], [s_.qd])
                    h.MM(pR.t[:], sel4.t[:, hh, :], c_.gcT.t[:], True, True, [sel4, c_.gcT], [pR])
                    yield
                    h.TR(ptr[0].t[:], s_.qn.t[:], G.identf.t[:], [s_.qn, G.identf], [ptr[0]])
                    h.TR(ptr[1].t[:], s_.kn.t[:], G.identf.t[:], [s_.kn, G.identf], [ptr[1]])
                    h.TR(ptr[2].t[:], s_.qd.t[:], G.identf.t[:], [s_.qd, G.identf], [ptr[2]])
                    h.CP("act", s_.qT.t[:], ptr[0].t[:], [ptr[0]], [s_.qT])
                    h.CP("act", s_.kT.t[:], ptr[1].t[:], [ptr[1]], [s_.kT])
                    h.CP("act", s_.qdT.t[:], ptr[2].t[:], [ptr[2]], [s_.qdT])
                    yield
                    h.TS("dve", s_.dtmp.t[:], pR.t[:], gch, 0.0, ALU.subtract, ALU.max, [pR, gcs], [s_.dtmp])
                    h.ACT(s_.dec.t[:], s_.dtmp.t[:], AF.Exp, [s_.dtmp], [s_.dec], scale=-1.0)
                    h.TT("pool", s_.dec.t[:], s_.dec.t[:], G.m_li.t[:], ALU.mult, [s_.dec, G.m_li], [s_.dec])
                    h.TS("dve", s_.dtmp.t[:], pR.t[:], gch, 0.0, ALU.subtract, ALU.min, [pR, gcs], [s_.dtmp])
                    h.ACT(s_.decT.t[:], s_.dtmp.t[:], AF.Exp, [s_.dtmp], [s_.decT])
                    h.TT("pool", s_.decT.t[:], s_.decT.t[:], G.m_ui.t[:], ALU.mult, [s_.decT, G.m_ui], [s_.decT])
                    yield
                    h.TS("dve", s_.kbg.t[:], s_.kn.t[:], c_.be.t[:, hh:hh + 1], None, ALU.mult, None, [s_.kn, c_.be], [s_.kbg])
                    h.TS("dve", s_.kdec.t[:], s_.kn.t[:], c_.ekd.t[:, hh:hh + 1], None, ALU.mult, None, [s_.kn, c_.ekd], [s_.kdec])
                    h.TS("dve", s_.vb.t[:], vs, beta.t[:, hh:hh + 1], None, ALU.mult, None, [qkv, beta], [s_.vb])
                    h.MM(pk.t[:], s_.kT.t[:], s_.kT.t[:], True, True, [s_.kT], [pk])
                    h.MM(pKQ.t[:], s_.kT.t[:], s_.qT.t[:], True, True, [s_.kT, s_.qT], [pKQ])
                    yield
                    h.STT("dve", s_.Lf.t[:], pk.t[:], beta.t[:, hh:hh + 1], s_.dec.t[:], ALU.mult, ALU.mult, [pk, beta, s_.dec], [s_.Lf])
                    h.TT("dve", s_.attnT.t[:], pKQ.t[:], s_.decT.t[:], ALU.mult, [pKQ, s_.decT], [s_.attnT])
                    h.TR(ptr[3].t[:], s_.Lf.t[:], G.identf.t[:], [s_.Lf, G.identf], [ptr[3]])
                    h.CP("act", s_.LT.t[:], ptr[3].t[:], [ptr[3]], [s_.LT])
                    yield
                    Dm = s_.Dm; DmT = s_.DmT; EkT = s_.EkT; Xs = s_.Xs; LT = s_.LT
                    h.TT("pool", Dm.t[:], s_.Lf.t[:], DB[0].t[:], ALU.mult, [s_.Lf, DB[0]], [Dm])
                    h.TT("pool", Dm.t[:], G.identf.t[:], Dm.t[:], ALU.subtract, [G.identf, Dm], [Dm])
                    h.TT("dve", DmT.t[:], LT.t[:], DB[0].t[:], ALU.mult, [LT, DB[0]], [DmT])
                    h.TT("dve", DmT.t[:], G.identf.t[:], DmT.t[:], ALU.subtract, [G.identf, DmT], [DmT])
                    yield
                    for k in range(1, 7):
                        h.TT("pool", EkT.t[:], LT.t[:], DB[k].t[:], ALU.mult, [LT, DB[k]], [EkT])
                        h.MM(pX.t[:], EkT.t[:], Dm.t[:], True, True, [EkT, Dm], [pX])
                        h.CP("act", Xs.t[:], pX.t[:], [pX], [Xs])
                        if k < 6:
                            h.MM(pY.t[:], DmT.t[:], Xs.t[:], True, True, [DmT, Xs], [pY])
                        h.MM(pYT.t[:], Xs.t[:], DmT.t[:], True, True, [Xs, DmT], [pYT])
                        if k < 6:
                            h.TT("dve", Dm.t[:], Dm.t[:], pY.t[:], ALU.subtract, [Dm, pY], [Dm])
                        h.TT("dve", DmT.t[:], DmT.t[:], pYT.t[:], ALU.subtract, [DmT, pYT], [DmT])
                        yield
                    h.MM(pU.t[:], DmT.t[:], s_.vb.t[:], True, True, [DmT, s_.vb], [pU])
                    h.MM(pW.t[:], s_.kbg.t[:], DmT.t[:], True, True, [s_.kbg, DmT], [pW])
                    h.CP("act", s_.us.t[:], pU.t[:], [pU], [s_.us])
                    h.CP("act", s_.wT.t[:], pW.t[:], [pW], [s_.wT])
                    yield
                    h.MM(pS1.t[:], s_.wT.t[:], Sb[hh].t[:], True, True, [s_.wT, Sb[hh]], [pS1])
                    h.TT("dve", s_.vnew.t[:], s_.us.t[:], pS1.t[:], ALU.subtract, [s_.us, pS1], [s_.vnew])
                    h.MM(pO.t[:], s_.qdT.t[:], Sb[hh].t[:], True, False, [s_.qdT, Sb[hh]], [pO])
                    h.MM(pO.t[:], s_.attnT.t[:], s_.vnew.t[:], False, True, [s_.attnT, s_.vnew], [pO])
                    h.MM(pSn.t[:], s_.kdec.t[:], s_.vnew.t[:], True, True, [s_.kdec, s_.vnew], [pSn])
                    yield
                    h.STT("dve", Sf[hh].t[:], Sf[hh].t[:], c_.egl.t[:, hh:hh + 1], pSn.t[:], ALU.mult, ALU.add, [Sf[hh], c_.egl, pSn], [Sf[hh]])
                    h.CP("pool", Sb[hh].t[:], Sf[hh].t[:], [Sf[hh]], [Sb[hh]])
                    oss = s_.oss
                    h.ACT(s_.ojunk.t[:], pO.t[:], AF.Square, [pO], [s_.ojunk, oss], accum_out=oss.t[:])
                    h.ACT(oss.t[:], oss.t[:], AF.Ln, [oss], [oss], scale=1.0 / 128, bias=EPS)
                    h.ACT(oss.t[:], oss.t[:], AF.Exp, [oss], [oss], scale=-0.5)
                    h.ACT(s_.on.t[:], pO.t[:], AF.Copy, [pO, oss], [s_.on], scale=oss.t[:])
                    h.TT("pool", c_.odn.t[:, hh * 128:(hh + 1) * 128], s_.on.t[:], c_.dgz.t[:, hh * 128:(hh + 1) * 128], ALU.mult, [s_.on, c_.dgz], [c_.odn])
                    yield

                def drain2(gens):
                    gens = list(gens)
                    while gens:
                        for g_ in list(gens):
                            try:
                                next(g_)
                            except StopIteration:
                                gens.remove(g_)
                drain2([prologue(0)])
                for n in range(nt if DNSTAGE > 0 else 0):
                    extra = [prologue(n + 1)] if n + 1 < nt else []
                    drain2([head(n, 0, 0), head(n, 1, 1)] + extra)
                    drain2([head(n, 2, 0), head(n, 3, 1)])
                    r0 = n * 128
                    h.DMA("sp", mixed.t[r0:r0 + 128, 0:512], C2[n % 2].odn.t[:], [C2[n % 2].odn], [mixed])
                kb.barrier()

        if upto >= 3 and 3 not in skip:
            with ExitStack() as pes:
                kb.es = pes
                gqk = h.tile("gqk", [128, 2, 64], F32)
                gout = h.tile("gout", [128, 64], F32)
                ones512 = h.tile("ones512", [128, 512], BF16)
                pes2 = ExitStack()
                kb.es = pes2
                sin = [h.tile("sin%d" % i, [128, 1536], F32) for i in range(2)]
                sq2 = h.tile("sq2", [128, 1024], F32)
                ssum = h.tile("ssum", [128, 16], F32)
                qkb = h.tile("qkb", [128, 1024], BF16)
                qkst = [h.tile("qkst%d" % i, [128, 8, 128], BF16) for i in range(2)]
                vst = [h.tile("vst%d" % i, [128, 512], BF16) for i in range(2)]
                pT = h.ptile("pT3", [128, 8, 128], BF16)
                h.DMA("sp", gqk.t[:, 0, :], sb_q_gain.to_broadcast([128, 64]), [], [gqk])
                h.DMA("sp", gqk.t[:, 1, :], sb_k_gain.to_broadcast([128, 64]), [], [gqk])
                h.DMA("sp", gout.t[:], sb_out_gain.to_broadcast([128, 64]), [], [gout])
                h.MEMSET("pool", ones512.t[:], 1.0, [ones512])
                for tt in range(nt):
                    X = sin[tt % 2]
                    QS = qkst[tt % 2]
                    VS = vst[tt % 2]
                    h.DMA("sp", X.t[:], proj.t[tt * 128:(tt + 1) * 128, 2056:3592], [proj], [X])
                    h.TT("dve", sq2.t[:], X.t[:, 0:1024], X.t[:, 0:1024], ALU.mult, [X], [sq2])
                    kb.op("dve", lambda e, X=X, ssum=ssum, sq2=sq2: e.tensor_reduce(out=ssum.t[:], in_=sq2.t[:].rearrange("p (a b) -> p a b", b=64),
                                                                 axis=AX.X, op=ALU.add), bl([sq2]), bl([ssum]))
                    h.ACT(ssum.t[:], ssum.t[:], AF.Ln, [ssum], [ssum], scale=1.0 / 64, bias=EPS)
                    h.ACT(ssum.t[:], ssum.t[:], AF.Exp, [ssum], [ssum], scale=-0.5)
                    h.TS("dve", ssum.t[:, 0:8], ssum.t[:, 0:8], 0.125, None, ALU.mult, None, [ssum], [ssum])
                    h.TT("dve", sq2.t[:].rearrange("p (a b) -> p a b", b=64), X.t[:, 0:1024].rearrange("p (a b) -> p a b", b=64),
                         ssum.t[:].unsqueeze(2).to_broadcast([128, 16, 64]), ALU.mult, [X, ssum], [sq2])
                    for qi in range(2):
                        h.TT("dve", qkb.t[:, qi * 512:(qi + 1) * 512].rearrange("p (a b) -> p a b", b=64),
                             sq2.t[:, qi * 512:(qi + 1) * 512].rearrange("p (a b) -> p a b", b=64),
                             gqk.t[:, qi, :].unsqueeze(1).to_broadcast([128, 8, 64]), ALU.mult, [sq2, gqk], [qkb])
                    for k in range(8):
                        h.TR(pT.t[:, k, :], qkb.t[:, k * 128:(k + 1) * 128], G.ident.t[:], [qkb, G.ident], [pT])
                    h.CP("act", QS.t[:], pT.t[:], [pT], [QS])
                    h.DMA("sp", qT_d.t[:, :, tt * 128:(tt + 1) * 128].rearrange("a p t -> p a t"), QS.t[:, 0:4, :], [QS], [qT_d])
                    h.DMA("sp", kT_d.t[:, :, tt * 128:(tt + 1) * 128].rearrange("a p t -> p a t"), QS.t[:, 4:8, :], [QS], [kT_d])
                    h.CP("act", VS.t[:], X.t[:, 1024:1536], [X], [VS])
                    h.DMA("sp", v_d.t[tt * 128:(tt + 1) * 128, :], VS.t[:], [VS], [v_d])
                kb.barrier()
                pes2.close()
                kb.es = pes
                NS = 4
                qT_hp = h.tile("qT_hp", [128, S], BF16)
                kT_hp = h.tile("kT_hp", [128, S], BF16)
                v_hp = h.tile("v_hp", [128, NT, 128], BF16)
                lpg = [h.tile("lpg%d" % sl, [128, 512], F32) for sl in range(NS)]
                ccg = [[h.tile("ccg%d_%d" % (sl, i), [128, 512], F32) for i in range(2)] for sl in range(NS)]
                ezs = [h.tile("ez%d" % sl, [128, 512], F32) for sl in range(NS)]
                zl2 = [h.tile("zl%d" % i, [128, S], F32) for i in range(NS)]
                arow2 = [h.tile("arow%d" % i, [128, S], BF16) for i in range(NS)]
                aT2 = [h.tile("aT%d" % i, [128, 8, 128], BF16) for i in range(NS)]
                ntots = [h.tile("ntot%d" % i, [128, 1], F32) for i in range(NS)]
                osss = [h.tile("oss%d" % i, [128, 1], F32) for i in range(NS)]
                ojunks = [h.tile("ojunk%d" % i, [128, 64], F32) for i in range(NS)]
                osb = [h.tile("osb%d" % i, [128, 128], BF16) for i in range(4)]
                pzs = [h.ptile("pz%d" % sl, [128, 512], F32) for sl in range(NS)]
                pAs = [h.ptile("pA%d" % i, [128, 8, 128], BF16) for i in range(NS)]
                done_cnt = {}

                def sb_head(hp, i, h2, sl):
                    hh = hp * 2 + h2
                    t1 = (i + 1) * 128
                    ngrp = (t1 + 511) // 512
                    OS = osb[(hp * nt + i) % 4]
                    p0 = h2 * 64
                    zl = zl2[sl]; arow = arow2[sl]; A8 = aT2[sl]
                    ntot = ntots[sl]; oss = osss[sl]; ojunk = ojunks[sl]
                    P = pzs[sl]; E = ezs[sl]; LP = lpg[sl]
                    for g in range(ngrp):
                        k0 = g * 512
                        kw = min(512, t1 - k0)
                        CC = ccg[sl][g % 2]
                        h.MM(P.t[:, 0:kw], qT_hp.t[p0:p0 + 64, i * 128:(i + 1) * 128], kT_hp.t[p0:p0 + 64, k0:k0 + kw],
                             True, True, [qT_hp, kT_hp], [P])
                        h.ACT(E.t[:, 0:kw], P.t[:, 0:kw], AF.Exp, [P], [E])
                        h.ACT(LP.t[:, 0:kw], E.t[:, 0:kw], AF.Ln, [E], [LP], bias=1.0)
                        h.TT("dve", zl.t[:, k0:k0 + kw], P.t[:, 0:kw], LP.t[:, 0:kw], ALU.subtract, [P, LP], [zl])
                        if g == ngrp - 1:
                            h.TT("pool", LP.t[:, kw - 128:kw], LP.t[:, kw - 128:kw], G.m_ls.t[:], ALU.mult, [LP, G.m_ls], [LP])
                        PC = ccg[sl][(g - 1) % 2]
                        init = 0.0 if g == 0 else PC.t[:, 511:512]
                        rd = [ones512, LP] + ([PC] if g > 0 else [])
                        kb.op("dve", lambda e, kw=kw, init=init, CC=CC, ones512=ones512, LP=LP: e.tensor_tensor_scan(
                            out=CC.t[:, 0:kw], data0=ones512.t[:, 0:kw], data1=LP.t[:, 0:kw], initial=init,
                            op0=ALU.mult, op1=ALU.add), bl(rd), bl([CC]))
                        h.TT("pool", zl.t[:, k0:k0 + kw], zl.t[:, k0:k0 + kw], CC.t[:, 0:kw], ALU.add, [zl, CC], [zl])
                        yield
                    kwl = t1 - (ngrp - 1) * 512
                    CL = ccg[sl][(ngrp - 1) % 2]
                    h.TS("dve", ntot.t[:], CL.t[:, kwl - 1:kwl], -1.0, None, ALU.mult, None, [CL], [ntot])
                    h.ACT(arow.t[:, 0:t1], zl.t[:, 0:t1], AF.Exp, [zl, ntot], [arow], bias=ntot.t[:])
                    h.TT("pool", arow.t[:, i * 128:t1], arow.t[:, i * 128:t1], G.m_ls.t[:], ALU.mult, [arow, G.m_ls], [arow])
                    yield
                    PA = pAs[sl]
                    PO = P
                    for b0 in range(0, i + 1, 8):
                        nb = min(8, i + 1 - b0)
                        for bb in range(nb):
                            h.TR(PA.t[:, bb, :], arow.t[:, (b0 + bb) * 128:(b0 + bb + 1) * 128], G.ident.t[:], [arow, G.ident], [PA])
                        h.CP("act", A8.t[:, 0:nb, :], PA.t[:, 0:nb, :], [PA], [A8])
                        for bb in range(nb):
                            blk = b0 + bb
                            h.MM(PO.t[:, 0:64], A8.t[:, bb, :], v_hp.t[:, blk, h2 * 64:(h2 + 1) * 64], blk == 0, blk == i, [A8, v_hp], [PO])
                        yield
                    h.ACT(ojunk.t[:], PO.t[:, 0:64], AF.Square, [PO], [ojunk, oss], accum_out=oss.t[:])
                    h.ACT(oss.t[:], oss.t[:], AF.Ln, [oss], [oss], scale=1.0 / 64, bias=EPS)
                    h.ACT(oss.t[:], oss.t[:], AF.Exp, [oss], [oss], scale=-0.5)
                    h.STT("dve", OS.t[:, h2 * 64:(h2 + 1) * 64], PO.t[:, 0:64], oss.t[:], gout.t[:], ALU.mult, ALU.mult, [PO, oss, gout], [OS])
                    key = (hp, i)
                    done_cnt[key] = done_cnt.get(key, 0) + 1
                    if done_cnt[key] == 2:
                        h.DMA("sp", mixed.t[i * 128:(i + 1) * 128, 512 + hp * 128:512 + (hp + 1) * 128], OS.t[:], [OS], [mixed])

                def load_pair(hp):
                    h.DMA("sp", qT_hp.t[:], qT_d.t[hp], [qT_d], [qT_hp])
                    h.DMA("sp", kT_hp.t[:], kT_d.t[hp], [kT_d], [kT_hp])
                    h.DMA("sp", v_hp.t[:], v_d.t[:, hp * 128:(hp + 1) * 128].rearrange("(b p) c -> p b c", p=128), [v_d], [v_hp])

                for hp in range(4):
                    load_pair(hp)
                    order = []
                    lo_, hi_ = 0, nt - 1
                    while lo_ <= hi_:
                        order.append(hi_)
                        if lo_ != hi_:
                            order.append(lo_)
                        lo_ += 1
                        hi_ -= 1
                    tasks = iter([(hp, i, h2) for i in order for h2 in range(2)])
                    active = []
                    free_slots = list(range(NS - 1, -1, -1))
                    while True:
                        while len(active) < NS:
                            t_ = next(tasks, None)
                            if t_ is None:
                                break
                            sl = free_slots.pop()
                            active.append((sb_head(t_[0], t_[1], t_[2], sl), sl))
                        if not active:
                            break
                        for item in list(active):
                            try:
                                next(item[0])
                            except StopIteration:
                                active.remove(item)
                                free_slots.append(item[1])
                kb.barrier()


        if upto >= 4 and 4 not in skip:
            with ExitStack() as pes:
                kb.es = pes
                woutb = h.tile("woutb", [128, 8, D], BF16)
                wqb = h.tile("wqb", [128, 8, 2048], BF16)
                keysT = h.tile("keysT", [128, 16, 128], BF16)
                pes2 = ExitStack()
                kb.es = pes2
                wst = [h.tile("wst4_%d" % i, [128, 8, 512], F32) for i in range(2)]
                pkT = h.ptile("pkT", [128, 4, 128], F32)
                wov = w_out.rearrange("(k p) n -> p k n", p=128)
                wqv = peer_w_query.rearrange("(k p) n -> p k n", p=128)
                for cg in range(6):
                    W_ = wst[cg % 2]
                    if cg < 2:
                        h.DMA("sp", W_.t[:], wov[:, :, cg * 512:(cg + 1) * 512], [], [W_])
                        h.CP("dve" if cg % 2 else "pool", woutb.t[:, :, cg * 512:(cg + 1) * 512], W_.t[:], [W_], [woutb])
                    else:
                        c0 = (cg - 2) * 512
                        h.DMA("sp", W_.t[:], wqv[:, :, c0:c0 + 512], [], [W_])
                        h.CP("dve" if cg % 2 else "pool", wqb.t[:, :, c0:c0 + 512], W_.t[:], [W_], [wqb])
                ksv = peer_sub_keys.rearrange("a n k -> n a k")
                for g4 in range(4):
                    W_ = wst[g4 % 2]
                    ksf = W_.t[:, 0:4, 0:128]
                    h.DMA("sp", ksf, ksv[:, g4 * 4:(g4 + 1) * 4, :], [], [W_])
                    for j in range(4):
                        h.TR(pkT.t[:, j, :], W_.t[:, j, 0:128], G.identf.t[:], [W_, G.identf], [pkT])
                    h.CP("act", keysT.t[:, g4 * 4:(g4 + 1) * 4, :], pkT.t[:], [pkT], [keysT])
                kb.barrier()
                pes2.close()
                kb.es = pes
                mx = [h.tile("mx%d" % i, [128, D], BF16) for i in range(2)]
                xt = [h.tile("xt4_%d" % i, [128, D], F32) for i in range(2)]
                junk_l = [h.tile("junk4_%d" % i, [128, D], F32) for i in range(2)]
                hb_l = [h.tile("hb4_%d" % i, [128, D], BF16) for i in range(2)]
                ss_l = [h.tile("ss4_%d" % i, [128, 1], F32) for i in range(2)]
                rstd_l = [h.tile("rstd4_%d" % i, [128, 1], F32) for i in range(2)]
                h2T_l = [h.tile("h2T%d" % i, [128, 8, 128], BF16) for i in range(2)]
                x1t_l = [h.tile("x1t%d" % i, [128, D], F32) for i in range(2)]
                mT_l = [h.tile("mT%d" % i, [128, 8, 128], BF16) for i in range(2)]
                qTs = [h.tile("qTs%d" % i, [128, 128], BF16) for i in range(2)]
                scl = [h.tile("sc%d" % i, [128, 16, 128], F32) for i in range(2)]
                pT = h.ptile("pT4", [128, 8, 128], BF16)
                pyo = [h.ptile("pyo%d" % i, [128, 512], F32) for i in range(2)]
                pq = [h.ptile("pq%d" % i, [128, 128], F32) for i in range(2)]
                psc = h.ptile("psc", [128, 4, 128], F32)
                wuf = [h.tile("wuf%d" % i, [128, D], F32) for i in range(2)]
                wvf = [h.tile("wvf%d" % i, [128, D], F32) for i in range(2)]
                wub = [h.tile("wub%d" % i, [128, 8, 128], BF16) for i in range(2)]
                wvb = [h.tile("wvb%d" % i, [128, D], BF16) for i in range(2)]
                ptw = [h.ptile("ptw%d" % i, [128, 4, 128], F32) for i in range(2)]

                def prep_load(c_):
                    h.DMA("sp", wuf[c_ % 2].t[:], peer_w_u[c_ * 128:(c_ + 1) * 128, :], [], [wuf[c_ % 2]])
                    h.DMA("sp", wvf[c_ % 2].t[:], peer_w_v[c_ * 128:(c_ + 1) * 128, :], [], [wvf[c_ % 2]])

                def prep_gen():
                    prep_load(0)
                    for c_ in range(NCH):
                        WU = wuf[c_ % 2]; WV = wvf[c_ % 2]; UB = wub[c_ % 2]; VB = wvb[c_ % 2]
                        for hf in range(2):
                            P_ = ptw[hf]
                            for j in range(4):
                                k = hf * 4 + j
                                h.TR(P_.t[:, j, :], WU.t[:, k * 128:(k + 1) * 128], G.identf.t[:], [WU, G.identf], [P_])
                            h.CP("act" if hf else "dve", UB.t[:, hf * 4:(hf + 1) * 4, :], P_.t[:], [P_], [UB])
                        h.DMA("pool", wuT_d.t[c_], UB.t[:], [UB], [wuT_d])
                        h.CP("pool", VB.t[:], WV.t[:], [WV], [VB])
                        h.DMA("pool", wv_d.t[c_], VB.t[:], [VB], [wv_d])
                        yield
                        if c_ + 1 < NCH:
                            prep_load(c_ + 1)
                prep_it = prep_gen() if (upto >= 5 and 5 not in skip) else iter(())
                def p4_tile(tt):
                    for _ in range((NCH + nt - 1) // nt):
                        next(prep_it, None)
                    r0 = tt * 128
                    SC = scl[tt % 2]
                    MXt = mx[tt % 2]
                    h2T = h2T_l[tt % 2]; x1t = x1t_l[tt % 2]; mT = mT_l[tt % 2]
                    X = xt[tt % 2]
                    h.DMA("sp", MXt.t[:], mixed.t[r0:r0 + 128, :], [mixed], [MXt])
                    h.DMA("sp", X.t[:], x[r0:r0 + 128, :], [], [X])
                    for k in range(8):
                        h.TR(pT.t[:, k, :], MXt.t[:, k * 128:(k + 1) * 128], G.ident.t[:], [MXt, G.ident], [pT])
                    h.CP("act", mT.t[:], pT.t[:], [pT], [mT])
                    for hf in range(2):
                        for k in range(8):
                            h.MM(pyo[hf].t[:], mT.t[:, k, :], woutb.t[:, k, hf * 512:(hf + 1) * 512], k == 0, k == 7, [mT, woutb], [pyo[hf]])
                        h.TT("dve", x1t.t[:, hf * 512:(hf + 1) * 512], pyo[hf].t[:], G.gate1.t[:, hf * 512:(hf + 1) * 512], ALU.mult, [pyo[hf], G.gate1], [x1t])
                    h.TT("pool", x1t.t[:], x1t.t[:], X.t[:], ALU.add, [x1t, X], [x1t])
                    h.DMA("sp", x1.t[r0:r0 + 128, :], x1t.t[:], [x1t], [x1])
                    yield
                    norm_mod_T(x1t, G.G2, G.S2, junk_l[tt % 2], hb_l[tt % 2], ss_l[tt % 2], rstd_l[tt % 2], pT, h2T)
                    h.DMA("sp", h2T_d.t[:, :, r0:r0 + 128], h2T.t[:], [h2T], [h2T_d])
                    yield
                    for hp in range(16):
                        PQ = pq[hp % 2]
                        QT = qTs[hp % 2]
                        for k in range(8):
                            h.MM(PQ.t[:], wqb.t[:, k, hp * 128:(hp + 1) * 128], h2T.t[:, k, :], k == 0, k == 7, [wqb, h2T], [PQ])
                        h.CP("act", QT.t[:], PQ.t[:], [PQ], [QT])
                        h.MM(psc.t[:, hp % 4, :], QT.t[:], keysT.t[:, hp, :], True, True, [QT, keysT], [psc])
                        if hp % 4 == 3:
                            h.CP("dve", SC.t[:, hp - 3:hp + 1, :], psc.t[:], [psc], [SC])
                            yield
                    h.DMA("sp", sc_d.t[r0:r0 + 128], SC.t[:], [SC], [sc_d])
                run_window([(lambda tt=tt: p4_tile(tt)) for tt in range(nt)], 2)
                kb.barrier()

        if upto >= 4 and 4 not in skip:
            with ExitStack() as pes:
                kb.es = pes
                sc_halves = [G.mod[0].t[:].rearrange("p (a b) -> p a b", b=128), G.mod[1].t[:].rearrange("p (a b) -> p a b", b=128)]
                sc_buf = kb.buf("sc4b")

                class _SC:
                    pass
                scw = _SC()
                scw.b = sc_buf
                scw.row = lambda hp: sc_halves[hp // 8][:, hp % 8, :]
                svl = [Tl(G.mod[2].t[:, i * 256:(i + 1) * 256].rearrange("p (a b) -> p a b", b=16), kb.buf()) for i in range(2)]
                wk = Tl(G.mod[2].t[:, 512:640], kb.buf())
                wk2 = Tl(G.mod[2].t[:, 640:896], kb.buf())
                cml = [h.tile("cm%d" % i, [128, 8, 16], F32) for i in range(2)]
                zz = h.tile("zz", [128, 8], F32)
                nbl = [h.tile("nbias%d" % i, [128, 8], F32) for i in range(2)]
                sm_l = [h.tile("sm%d" % i, [128, 16, 128], F32) for i in range(1)]
                ee_l = [h.tile("ee%d" % i, [128, 16, 128], F32) for i in range(1)]
                cand = Tl(sm_l[0].t[:].rearrange("p (a c) b -> p a (c b)", a=8), sm_l[0].b)
                ce = Tl(ee_l[0].t[:].rearrange("p (a c) b -> p a (c b)", a=8), ee_l[0].b)
                Gp = h.tile("Gp", [128, 8, 16, 128], BF16)
                OH = h.tile("OH", [128, 8, 16, 128], BF16)
                GpT = h.tile("GpT", [128, 128, 128], BF16)
                OHT = h.tile("OHT", [128, 128, 128], BF16)
                GTt = h.tile("GTt", [128, 128, 128], BF16)
                pTr = [h.ptile("pTr%d" % i, [128, 8, 128], BF16) for i in range(2)]
                pG4 = [h.ptile("pG4_%d" % i, [128, 4, 128], F32) for i in range(2)]

                def genA(tt):
                    r0 = tt * 128
                    sc = scw; sv = svl[tt % 2]; cm = cml[tt % 2]; nbias = nbl[tt % 2]
                    h.DMA("sp", sc_halves[0], sc_d.t[r0:r0 + 128, 0:8, :], [sc_d], [sc])
                    h.DMA("sp", sc_halves[1], sc_d.t[r0:r0 + 128, 8:16, :], [sc_d], [sc])
                    for hp in range(16):
                        kb.op("dve", lambda e, hp=hp, sv=sv, sc=sc: e.max(out=sv.t[:, hp, 0:8], in_=sc.row(hp)), bl([sc]), bl([sv]))
                        kb.op("dve", lambda e, hp=hp, sv=sv, sc=sc, wk=wk: e.match_replace(out=wk.t[:], in_to_replace=sv.t[:, hp, 0:8], in_values=sc.row(hp), imm_value=-1e30),
                              bl([sc, sv]), bl([wk]))
                        kb.op("dve", lambda e, hp=hp, sv=sv, wk=wk: e.max(out=sv.t[:, hp, 8:16], in_=wk.t[:]), bl([wk]), bl([sv]))
                        if hp % 4 == 3:
                            yield
                    sv4 = sv.t[:].rearrange("p (a b) k -> p a b k", b=2)
                    h.TT("dve", cand.t[:].rearrange("p a (i j) -> p a i j", j=16),
                         sv4[:, :, 0, :].unsqueeze(3).to_broadcast([128, 8, 16, 16]),
                         sv4[:, :, 1, :].unsqueeze(2).to_broadcast([128, 8, 16, 16]), ALU.add, [sv], [cand])
                    for hh in range(8):
                        kb.op("dve", lambda e, hh=hh, cm=cm, cand=cand: e.max(out=cm.t[:, hh, 0:8], in_=cand.t[:, hh, :]), bl([cand]), bl([cm]))
                        kb.op("dve", lambda e, hh=hh, cm=cm, cand=cand, wk2=wk2: e.match_replace(out=wk2.t[:], in_to_replace=cm.t[:, hh, 0:8], in_values=cand.t[:, hh, :], imm_value=-1e30),
                              bl([cand, cm]), bl([wk2]))
                        kb.op("dve", lambda e, hh=hh, cm=cm, wk2=wk2: e.max(out=cm.t[:, hh, 8:16], in_=wk2.t[:]), bl([wk2]), bl([cm]))
                        if hh % 4 == 3:
                            yield
                    h.TT("dve", ce.t[:], cand.t[:], cm.t[:, :, 0:1].to_broadcast([128, 8, 256]), ALU.subtract, [cand, cm], [ce])
                    h.ACT(ce.t[:], ce.t[:], AF.Exp, [ce], [ce])
                    h.TT("dve", cand.t[:], cand.t[:], cm.t[:, :, 15:16].to_broadcast([128, 8, 256]), ALU.is_ge, [cand, cm], [cand])
                    h.TT("dve", ce.t[:], ce.t[:], cand.t[:], ALU.mult, [ce, cand], [ce])
                    kb.op("dve", lambda e, zz=zz, ce=ce: e.tensor_reduce(out=zz.t[:], in_=ce.t[:], axis=AX.X, op=ALU.add), bl([ce]), bl([zz]))
                    h.ACT(zz.t[:], zz.t[:], AF.Ln, [zz], [zz])
                    h.TT("dve", nbias.t[:], zz.t[:], cm.t[:, :, 0], ALU.add, [zz, cm], [nbias])
                    h.TS("dve", nbias.t[:], nbias.t[:], -1.0, None, ALU.mult, None, [nbias], [nbias])
                    yield
                    for hh in range(8):
                        sm = sm_l[0]; ee = ee_l[0]
                        h.TT("pool", sm.t[:], sv.t[:, 2 * hh, :].unsqueeze(2).to_broadcast([128, 16, 128]),
                             sc.row(2 * hh + 1).unsqueeze(1).to_broadcast([128, 16, 128]), ALU.add, [sv, sc], [sm])
                        h.ACT(ee.t[:], sm.t[:], AF.Exp, [sm, nbias], [ee], bias=nbias.t[:, hh:hh + 1])
                        h.STT("dve", Gp.t[:, hh, :, :], sm.t[:], cm.t[:, hh, 15:16], ee.t[:], ALU.is_ge, ALU.mult, [sm, cm, ee], [Gp])
                        h.TT("dve", OH.t[:, hh, :, :], sc.row(2 * hh).unsqueeze(1).to_broadcast([128, 16, 128]),
                             sv.t[:, 2 * hh, :].unsqueeze(2).to_broadcast([128, 16, 128]), ALU.is_equal, [sc, sv], [OH])
                        yield

                def genT(tt):
                    ntr = 0
                    for (src, dst) in ((Gp, GpT), (OH, OHT)):
                        for g8 in range(16):
                            PT = pTr[ntr % 2]
                            ev_eng = "act" if ntr % 2 == 0 else "dve"
                            ntr += 1
                            for q in range(8):
                                ii = g8 * 8 + q
                                h.TR(PT.t[:, q, :], src.t[:, :, :, ii].rearrange("p a b -> p (a b)"), G.ident.t[:], [src, G.ident], [PT])
                            h.CP(ev_eng, dst.t[:, g8 * 8:(g8 + 1) * 8, :], PT.t[:], [PT], [dst])
                            yield

                def genB(tt):
                    r0 = tt * 128
                    for t4 in range(32):
                        PG = pG4[t4 % 2]
                        for q in range(4):
                            tl = t4 * 4 + q
                            h.MM(PG.t[:, q, :], GpT.t[:, :, tl], OHT.t[:, :, tl], True, True, [GpT, OHT], [PG])
                        tok0 = t4 * 4
                        h.CP("act", GTt.t[:, :, tok0:tok0 + 4], PG.t[:].rearrange("p t c -> p c t"), [PG], [GTt])
                        if t4 % 2 == 1:
                            yield
                    for c4 in range(4):
                        h.DMA("sp", GT_d.t[c4 * 32:(c4 + 1) * 32, :, r0:r0 + 128].rearrange("c i t -> i c t"), GTt.t[:, c4 * 32:(c4 + 1) * 32, :], [GTt], [GT_d])

                def drain(gens):
                    gens = list(gens)
                    while gens:
                        for g_ in list(gens):
                            try:
                                next(g_)
                            except StopIteration:
                                gens.remove(g_)
                drain([genA(0)])
                drain([genT(0)])
                for tt in range(nt):
                    if tt + 1 < nt:
                        drain([genA(tt + 1), genB(tt)])
                        drain([genT(tt + 1)])
                    else:
                        drain([genB(tt)])
                kb.barrier()

        if upto >= 5 and 5 not in skip:
            with ExitStack() as pes:
                kb.es = pes
                TG = 256
                CB = 16
                NB = NCH // CB
                h2g = [h.tile("h2g%d" % i, [128, 8, TG], BF16) for i in range(2)]
                wu8 = [h.tile("wu8_%d" % i, [128, CB, 8, 128], BF16) for i in range(2)]
                wv8 = [h.tile("wv8_%d" % i, [128, CB, D], BF16) for i in range(2)]
                gtc = [h.tile("gtc%d" % i, [128, TG], BF16) for i in range(8)]
                gl = [h.tile("gl%d" % i, [128, TG], F32) for i in range(2)]
                actb = [h.tile("actb%d" % i, [128, TG], BF16) for i in range(10)]
                ysb = [[h.tile("ysb%d_%d" % (a, b), [128, D], F32) for b in range(2)] for a in range(2)]
                x1r = [h.tile("x1r%d" % i, [128, D], F32) for i in range(2)]
                ppre = [h.ptile("ppre%d" % i, [128, 512], F32) for i in range(2)]
                pyy = [[h.ptile("pyy%d_%d" % (a, b), [128, 512], F32) for b in range(2)] for a in range(2)]
                wuv = wuT_d.t.rearrange("c d k e -> d c k e")
                wvv = wv_d.t.rearrange("c e n -> e c n")
                for tp in range(nt // 4):
                    for tg2 in range(2):
                        t0 = (tp * 2 + tg2) * TG
                        h.DMA("sp", h2g[tg2].t[:], h2T_d.t[:, :, t0:t0 + TG], [h2T_d], [h2g[tg2]])
                    its = [(cb, tg2, cl) for cb in range(NB) for tg2 in range(2) for cl in range(CB)]
                    prev_batch = []
                    cur_batch = []
                    n_it = 0

                    def finish(pv):
                        cb, tg2, cl, AB = pv
                        WV = wv8[cb % 2]
                        for t2 in range(2):
                            for dh in range(2):
                                h.MM(pyy[t2][dh].t[:], AB.t[:, t2 * 128:(t2 + 1) * 128], WV.t[:, cl, dh * 512:(dh + 1) * 512],
                                     cl == 0, cl == CB - 1, [AB, WV], [pyy[t2][dh]])
                        if cl == CB - 1:
                            for t2 in range(2):
                                Y = ysb[tg2][t2]
                                for dh in range(2):
                                    if cb == 0:
                                        h.CP("dve", Y.t[:, dh * 512:(dh + 1) * 512], pyy[t2][dh].t[:], [pyy[t2][dh]], [Y])
                                    else:
                                        h.TT("dve", Y.t[:, dh * 512:(dh + 1) * 512], Y.t[:, dh * 512:(dh + 1) * 512], pyy[t2][dh].t[:], ALU.add,
                                             [Y, pyy[t2][dh]], [Y])
                    for (cb, tg2, cl) in its:
                        c_ = cb * CB + cl
                        t0 = (tp * 2 + tg2) * TG
                        WU = wu8[cb % 2]; WV = wv8[cb % 2]
                        if tg2 == 0 and cl == 0:
                            h.DMA("sp", WU.t[:], wuv[:, cb * CB:(cb + 1) * CB], [wuT_d], [WU])
                            h.DMA("pool", WV.t[:], wvv[:, cb * CB:(cb + 1) * CB], [wv_d], [WV])
                        GC = gtc[n_it % 8]
                        h.DMA("sp", GC.t[:], GT_d.t[c_, :, t0:t0 + TG], [GT_d], [GC])
                        PP = ppre[n_it % 2]; GL = gl[n_it % 2]; AB = actb[n_it % 10]
                        H2 = h2g[tg2]
                        for k in range(8):
                            h.MM(PP.t[:, 0:TG], WU.t[:, cl, k, :], H2.t[:, k, :], k == 0, k == 7, [WU, H2], [PP])
                        h.ACT(GL.t[:], PP.t[:, 0:TG], AF.Gelu, [PP], [GL])
                        h.TT("pool", AB.t[:], GL.t[:], GC.t[:], ALU.mult, [GL, GC], [AB])
                        cur_batch.append((cb, tg2, cl, AB))
                        n_it += 1
                        if len(cur_batch) == 4:
                            for pv in prev_batch:
                                finish(pv)
                            prev_batch = cur_batch
                            cur_batch = []
                    for pv in prev_batch + cur_batch:
                        finish(pv)
                    for tg2 in range(2):
                        for t2 in range(2):
                            r0 = (tp * 2 + tg2) * TG + t2 * 128
                            X1 = x1r[t2]; Y = ysb[tg2][t2]
                            h.DMA("sp", X1.t[:], x1.t[r0:r0 + 128, :], [x1], [X1])
                            h.TT("dve", Y.t[:], Y.t[:], G.gate2.t[:], ALU.mult, [Y, G.gate2], [Y])
                            h.TT("pool", Y.t[:], Y.t[:], X1.t[:], ALU.add, [Y, X1], [Y])
                            h.DMA("sp", OUT.t[r0:r0 + 128, :], Y.t[:], [Y], [OUT])
                kb.barrier()

        kb.final_wait("sp")
        kb.emit()
    return nc


_NC_CACHE = {}


def kernel(**inputs):
    n = 8
    if "nc" not in _NC_CACHE:
        _NC_CACHE["nc"] = build()
    nc = _NC_CACHE["nc"]
    x = np.ascontiguousarray(inputs["x"], dtype=np.float32)
    c = np.ascontiguousarray(inputs["c"], dtype=np.float32)
    shared = {}
    for k, v in inputs.items():
        if k in ("x", "c"):
            continue
        a = np.asarray(v, dtype=np.float32)[0]
        if k == "peer_sub_keys":
            a = a.reshape(16, 128, 128)
        elif k == "dn_conv_w":
            a = a.reshape(1, 6144)
        elif a.ndim == 1:
            a = a[None, :]
        shared[k] = np.ascontiguousarray(a)
    in_maps = []
    for b in range(n):
        m = dict(shared)
        m["x"] = x[b]
        m["c"] = c[b:b + 1]
        in_maps.append(m)
    res = run_bass_kernel_spmd(nc, in_maps, core_ids=list(range(n)))
    return np.stack([np.asarray(r["out"], dtype=np.float32) for r in res.results], axis=0)
```

```python
import numpy as np
import concourse.bass as bass
import concourse.mybir as mybir
from concourse.bass_utils import run_bass_kernel_spmd
from contextlib import ExitStack

F32 = mybir.dt.float32
BF16 = mybir.dt.bfloat16
U32 = mybir.dt.uint32
I32 = mybir.dt.int32
AF = mybir.ActivationFunctionType
ALU = mybir.AluOpType
AX = mybir.AxisListType

N_DMA_SEMS = 12
SAME_ENG_SYNC = True


class Buf:
    __slots__ = ("name", "last_w", "reads")

    def __init__(self, name):
        self.name = name
        self.last_w = []
        self.reads = []


class KB:
    def __init__(self, nc, es):
        self.nc = nc
        self.es = es
        self.engs = {"pe": nc.tensor, "act": nc.scalar, "dve": nc.vector, "pool": nc.gpsimd, "sp": nc.sync}
        self.q = {e: [] for e in self.engs}
        self.cnt = {}
        self.sems = {}
        self.waited = {e: {} for e in self.engs}
        for e in self.engs:
            self.sems[e] = es.enter_context(nc.semaphore("s_" + e))
            self.cnt[e] = 0
        self.dma_rr = {}
        for e in ("sp", "pool", "act"):
            for i in range(N_DMA_SEMS):
                k = "d_%s_%d" % (e, i)
                self.sems[k] = es.enter_context(nc.semaphore(k))
                self.cnt[k] = 0
            self.dma_rr[e] = 0
        self.nbuf = 0

    def sb(self, name, shape, dt):
        t = self.es.enter_context(self.nc.sbuf_tensor(name, list(shape), dt))
        return t

    def buf(self, name=None):
        self.nbuf += 1
        return Buf(name or ("b%d" % self.nbuf))

    def _waits(self, eng, reads, writes):
        deps = []
        for b in reads:
            deps += b.last_w
        for b in writes:
            deps += b.last_w
            deps += b.reads
        need = {}
        for (k, v) in deps:
            if k == eng and (eng == "pe" or eng == "sp" or (not SAME_ENG_SYNC and eng != "pool")):
                continue
            if need.get(k, 0) < v:
                need[k] = v
        out = []
        w = self.waited[eng]
        for k, v in need.items():
            if w.get(k, 0) >= v:
                continue
            w[k] = v
            out.append((k, v))
        return out

    def _commit(self, ev, reads, writes):
        for b in writes:
            b.last_w = [ev]
            b.reads = []
        for b in reads:
            if b not in writes:
                b.reads.append(ev)
                if len(b.reads) > 64:
                    mx = {}
                    for (k, v) in b.reads:
                        if mx.get(k, 0) < v:
                            mx[k] = v
                    b.reads = list(mx.items())

    def op(self, eng, fn, reads=(), writes=()):
        reads = list(reads)
        writes = list(writes)
        waits = self._waits(eng, reads, writes)
        self.cnt[eng] += 1
        ev = (eng, self.cnt[eng])
        self._commit(ev, reads, writes)
        self.q[eng].append((waits, fn, eng, 1))
        return ev

    def dma(self, eng, out, in_, reads=(), writes=(), **kw):
        reads = list(reads)
        writes = list(writes)
        waits = self._waits(eng, reads, writes)
        i = self.dma_rr[eng]
        self.dma_rr[eng] = (i + 1) % N_DMA_SEMS
        k = "d_%s_%d" % (eng, i)
        if self.cnt[k] > 0 and self.waited[eng].get(k, 0) < self.cnt[k]:
            self.waited[eng][k] = self.cnt[k]
            waits.append((k, self.cnt[k]))
        self.cnt[k] += 16
        ev = (k, self.cnt[k])
        self._commit(ev, reads, writes)
        self.q[eng].append((waits, (lambda e, o=out, i_=in_, kw=kw: e.dma_start(out=o, in_=i_, **kw)), k, 16))
        return ev

    def barrier(self):
        snap = dict(self.cnt)
        for eng in self.engs:
            waits = []
            w = self.waited[eng]
            for k, v in snap.items():
                if v == 0 or k == eng:
                    continue
                if w.get(k, 0) >= v:
                    continue
                w[k] = v
                waits.append((k, v))
            if waits:
                self.q[eng].append((waits, None, None, 0))

    def final_wait(self, eng="sp"):
        snap = dict(self.cnt)
        waits = [(k, v) for k, v in snap.items() if v > 0 and k != eng]
        self.q[eng].append((waits, None, None, 0))

    def emit(self):
        nc = self.nc
        sems = self.sems

        def replay(name, e):
            for (waits, fn, inck, incv) in self.q[name]:
                for (k, v) in waits:
                    e.wait_ge(sems[k], v)
                if fn is not None:
                    ins = fn(e)
                    ins.then_inc(sems[inck], incv)

        with nc.Block() as block:
            @block.tensor
            def _(e):
                replay("pe", e)

            @block.scalar
            def _(e):
                replay("act", e)

            @block.vector
            def _(e):
                replay("dve", e)

            @block.gpsimd
            def _(e):
                replay("pool", e)

            @block.sync
            def _(e):
                replay("sp", e)


import os
SBSTAGE = int(os.environ.get('SBSTAGE', '9'))
PREP = int(os.environ.get('PREP', '9'))
DNSTAGE = int(os.environ.get('DNSTAGE', '9'))

S = 4096
D = 1024
NT = 32
INC = 3592
EPS = 1e-6


class Tl:
    __slots__ = ("t", "b")

    def __init__(self, t, b):
        self.t = t
        self.b = b


def bl(ts):
    return [t.b for t in ts]


class Ctx:
    pass


def mk(kb):
    h = Ctx()

    def tile(name, shape, dt, es=None):
        t = (es or kb.es).enter_context(kb.nc.sbuf_tensor(name, list(shape), dt))
        return Tl(t, kb.buf(name))

    def ptile(name, shape, dt, es=None):
        t = (es or kb.es).enter_context(kb.nc.psum_tensor(name, list(shape), dt))
        return Tl(t, kb.buf(name))

    def ACT(out, in_, func, r, w, **kw):
        kb.op("act", lambda e: e.activation(out=out, in_=in_, func=func, **kw), bl(r), bl(w))

    def TT(eng, out, in0, in1, op, r, w):
        kb.op(eng, lambda e: e.tensor_tensor(out=out, in0=in0, in1=in1, op=op), bl(r), bl(w))

    def TS(eng, out, in0, s1, s2, op0, op1, r, w):
        if op1 is None:
            kb.op(eng, lambda e: e.tensor_scalar(out, in0, s1, None, op0), bl(r), bl(w))
        else:
            kb.op(eng, lambda e: e.tensor_scalar(out, in0, s1, s2, op0, op1), bl(r), bl(w))

    def STT(eng, out, in0, scalar, in1, op0, op1, r, w):
        kb.op(eng, lambda e: e.scalar_tensor_tensor(out=out, in0=in0, scalar=scalar, in1=in1, op0=op0, op1=op1), bl(r), bl(w))

    def CP(eng, out, in_, r, w):
        if eng == "act":
            kb.op("act", lambda e: e.activation(out=out, in_=in_, func=AF.Copy), bl(r), bl(w))
        else:
            kb.op(eng, lambda e: e.tensor_copy(out, in_), bl(r), bl(w))

    def MM(out, lhsT, rhs, start, stop, r, w):
        kb.op("pe", lambda e: e.matmul(out, lhsT=lhsT, rhs=rhs, start=start, stop=stop), bl(r), bl(w))

    def TR(out, in_, ident, r, w):
        kb.op("pe", lambda e: e.transpose(out, in_, ident), bl(r), bl(w))

    def DMA(eng, out, in_, r, w, **kw):
        kb.dma(eng, out, in_, bl(r), bl(w), **kw)

    def MEMSET(eng, ap, val, w):
        kb.op(eng, lambda e: e.memset(ap, val), [], bl(w))

    def ASEL(out, in_, pattern, base, cm, cmp, fill, r, w):
        kb.op("pool", lambda e: e.affine_select(out=out, in_=in_, pattern=pattern, base=base, channel_multiplier=cm,
                                                 compare_op=cmp, fill=fill), bl(r), bl(w))

    def RECIP(out, in_, r, w):
        kb.op("dve", lambda e: e.reciprocal(out, in_), bl(r), bl(w))

    for k, v in list(locals().items()):
        if k not in ("h", "kb"):
            setattr(h, k, v)
    return h


def build(dbg=(), nt=NT, upto=9, skip=()):
    nc = bass.Bass("TRN2", target_bir_lowering=False)
    dbg = set(dbg)

    def din(name, shape):
        return nc.dram_tensor(name, list(shape), F32, kind="ExternalInput").ap()

    def scratch(name, shape, dt):
        kind = "ExternalOutput" if name in dbg else "Internal"
        return Tl(nc.dram_tensor(name, list(shape), dt, kind=kind).ap(), Buf(name))

    x = din("x", [S, D])
    c = din("c", [1, D])
    w_ada = din("w_ada", [D, 6 * D])
    b_ada = din("b_ada", [1, 6 * D])
    norm1_gain = din("norm1_gain", [1, D])
    w_in = din("w_in", [D, INC])
    dn_conv_w = din("dn_conv_w", [1, 6144])
    dn_a_log = din("dn_a_log", [1, 4])
    dn_dt_bias = din("dn_dt_bias", [1, 4])
    dn_out_gain = din("dn_out_gain", [1, 128])
    sb_q_gain = din("sb_q_gain", [1, 64])
    sb_k_gain = din("sb_k_gain", [1, 64])
    sb_out_gain = din("sb_out_gain", [1, 64])
    w_out = din("w_out", [D, D])
    norm2_gain = din("norm2_gain", [1, D])
    peer_w_query = din("peer_w_query", [D, 2048])
    peer_sub_keys = din("peer_sub_keys", [16, 128, 128])
    peer_w_u = din("peer_w_u", [16384, D])
    peer_w_v = din("peer_w_v", [16384, D])
    out = nc.dram_tensor("out", [S, D], F32, kind="ExternalOutput").ap()
    OUT = Tl(out, Buf("out"))

    proj = scratch("proj", [S, INC], F32)
    mixed = scratch("mixed", [S, D], BF16)
    x1 = scratch("x1", [S, D], F32)
    moddbg = scratch("moddbg", [128, 6, D], F32) if "moddbg" in dbg else None
    NCH = int(os.environ.get("NCH", "128"))
    h2T_d = scratch("h2T_d", [128, 8, S], BF16)
    sc_d = scratch("sc_d", [S, 16, 128], F32)
    qT_d = scratch("qT_d", [4, 128, S], BF16)
    kT_d = scratch("kT_d", [4, 128, S], BF16)
    v_d = scratch("v_d", [S, 512], BF16)
    GT_d = scratch("GT_d", [128, 128, S], BF16)
    wuT_d = scratch("wuT_d", [128, 128, 8, 128], BF16)
    wv_d = scratch("wv_d", [128, 128, D], BF16)

    with ExitStack() as ges:
        kb = KB(nc, ges)
        h = mk(kb)
        G = Ctx()
        G.identf = h.tile("identf", [128, 128], F32)
        G.ident = h.tile("ident", [128, 128], BF16)
        G.onesf = h.tile("onesf", [128, 128], F32)
        G.m_li = h.tile("m_li", [128, 128], F32)
        G.m_ls = h.tile("m_ls", [128, 128], F32)
        G.m_ui = h.tile("m_ui", [128, 128], F32)
        G.m_us = h.tile("m_us", [128, 128], F32)
        G.mod = [h.tile("mod%d" % i, [128, D], F32) for i in range(6)]
        h.MEMSET("pool", G.identf.t[:], 0.0, [G.identf])
        h.ASEL(G.identf.t[:], G.identf.t[:], [[-1, 128]], 0, 1, ALU.not_equal, 1.0, [G.identf], [G.identf])
        h.CP("dve", G.ident.t[:], G.identf.t[:], [G.identf], [G.ident])
        h.MEMSET("pool", G.onesf.t[:], 1.0, [G.onesf])
        h.ASEL(G.m_li.t[:], G.onesf.t[:], [[-1, 128]], 0, 1, ALU.is_ge, 0.0, [G.onesf], [G.m_li])
        h.ASEL(G.m_ls.t[:], G.onesf.t[:], [[-1, 128]], 0, 1, ALU.is_gt, 0.0, [G.onesf], [G.m_ls])
        h.ASEL(G.m_ui.t[:], G.onesf.t[:], [[1, 128]], 0, -1, ALU.is_ge, 0.0, [G.onesf], [G.m_ui])
        h.ASEL(G.m_us.t[:], G.onesf.t[:], [[1, 128]], 0, -1, ALU.is_gt, 0.0, [G.onesf], [G.m_us])

        with ExitStack() as pes:
            kb.es = pes
            c_sb = h.tile("c_sb", [128, 8], F32)
            crep = h.tile("crep", [128, 8, 128], F32)
            brow = h.tile("brow", [1, 6 * D], F32)
            wa = [h.tile("wa%d" % i, [128, 8, 512], F32) for i in range(2)]
            ng = [h.tile("ng%d" % i, [128, D], F32) for i in range(2)]
            pm = [h.ptile("pm%d" % i, [128, 512], F32) for i in range(2)]
            h.DMA("sp", c_sb.t[:], c.rearrange("o (k p) -> p (o k)", p=128), [], [c_sb], allow_slow_non_contiguous=True)
            h.DMA("sp", brow.t[:], b_ada, [], [brow])
            h.DMA("sp", ng[0].t[:], norm1_gain.to_broadcast([128, D]), [], [ng[0]])
            h.DMA("sp", ng[1].t[:], norm2_gain.to_broadcast([128, D]), [], [ng[1]])
            h.ACT(c_sb.t[:], c_sb.t[:], AF.Silu, [c_sb], [c_sb])
            h.CP("dve", crep.t[:], c_sb.t[:].unsqueeze(2).to_broadcast([128, 8, 128]), [c_sb], [crep])
            wav = w_ada.rearrange("(k p) n -> p k n", p=128)
            for cg in range(12):
                w_ = wa[cg % 2]
                p_ = pm[cg % 2]
                h.DMA("sp", w_.t[:], wav[:, :, cg * 512:(cg + 1) * 512], [], [w_])
                for k in range(8):
                    h.MM(p_.t[:], crep.t[:, k, :], w_.t[:, k, :], k == 0, False, [crep, w_], [p_])
                h.MM(p_.t[:], G.onesf.t[0:1, :], brow.t[0:1, cg * 512:(cg + 1) * 512], False, True, [G.onesf, brow], [p_])
                m_ = G.mod[cg // 2]
                h.CP("act" if cg % 2 else "dve", m_.t[:, (cg % 2) * 512:(cg % 2 + 1) * 512], p_.t[:], [p_], [m_])
            if moddbg is not None:
                for i in range(6):
                    h.DMA("sp", moddbg.t[:, i, :], G.mod[i].t[:], [G.mod[i]], [moddbg])
            h.STT("dve", G.mod[1].t[:], G.mod[1].t[:], 1.0, ng[0].t[:], ALU.add, ALU.mult, [G.mod[1], ng[0]], [G.mod[1]])
            h.STT("dve", G.mod[4].t[:], G.mod[4].t[:], 1.0, ng[1].t[:], ALU.add, ALU.mult, [G.mod[4], ng[1]], [G.mod[4]])
            kb.barrier()
        G.S1, G.G1, G.gate1, G.S2, G.G2, G.gate2 = G.mod

        def run_window(gen_fns, width):
            it = iter(gen_fns)
            active = []
            while True:
                while len(active) < width:
                    f = next(it, None)
                    if f is None:
                        break
                    active.append(f())
                if not active:
                    break
                for g_ in list(active):
                    try:
                        next(g_)
                    except StopIteration:
                        active.remove(g_)

        def norm_mod_T(xt, Gt, St, junk, hb, ss, rstd, pT, hT):
            h.ACT(junk.t[:], xt.t[:], AF.Square, [xt], [junk, ss], accum_out=ss.t[:])
            h.ACT(rstd.t[:], ss.t[:], AF.Ln, [ss], [rstd], scale=1.0 / D, bias=EPS)
            h.ACT(rstd.t[:], rstd.t[:], AF.Exp, [rstd], [rstd], scale=-0.5)
            h.STT("dve", junk.t[:], xt.t[:], rstd.t[:], Gt.t[:], ALU.mult, ALU.mult, [xt, rstd, Gt], [junk])
            h.TT("pool", hb.t[:], junk.t[:], St.t[:], ALU.add, [junk, St], [hb])
            for k in range(8):
                h.TR(pT.t[:, k, :], hb.t[:, k * 128:(k + 1) * 128], G.ident.t[:], [hb, G.ident], [pT])
            h.CP("act", hT.t[:], pT.t[:], [pT], [hT])

        if upto >= 1:
            with ExitStack() as pes:
                kb.es = pes
                winb = h.tile("winb", [128, 8, INC], BF16)
                wst = [h.tile("wst%d" % i, [128, 8, 512], F32) for i in range(2)]
                xt = [h.tile("xt%d" % i, [128, D], F32) for i in range(2)]
                junk_l = [h.tile("junk%d" % i, [128, D], F32) for i in range(2)]
                hb_l = [h.tile("hb%d" % i, [128, D], BF16) for i in range(2)]
                ss_l = [h.tile("ss%d" % i, [128, 1], F32) for i in range(2)]
                rstd_l = [h.tile("rstd%d" % i, [128, 1], F32) for i in range(2)]
                hT = [h.tile("hT%d" % i, [128, 8, 128], BF16) for i in range(2)]
                yt = [h.tile("yt%d" % i, [128, INC], F32) for i in range(2)]
                pT = h.ptile("pT", [128, 8, 128], BF16)
                py = [h.ptile("py%d" % i, [128, 512], F32) for i in range(3)]
                wiv = w_in.rearrange("(k p) n -> p k n", p=128)
                for cg in range(8):
                    c0 = cg * 512
                    w = min(512, INC - c0)
                    h.DMA("sp", wst[cg % 2].t[:, :, 0:w], wiv[:, :, c0:c0 + w], [], [wst[cg % 2]])
                    h.CP("dve" if cg % 2 else "pool", winb.t[:, :, c0:c0 + w], wst[cg % 2].t[:, :, 0:w], [wst[cg % 2]], [winb])
                def p1_tile(tt):
                    X = xt[tt % 2]
                    h.DMA("sp", X.t[:], x[tt * 128:(tt + 1) * 128, :], [], [X])
                    HT = hT[tt % 2]
                    norm_mod_T(X, G.G1, G.S1, junk_l[tt % 2], hb_l[tt % 2], ss_l[tt % 2], rstd_l[tt % 2], pT, HT)
                    yield
                    Y = yt[tt % 2]
                    for cg in range(8):
                        c0 = cg * 512
                        w = min(512, INC - c0)
                        P = py[cg % 3]
                        for k in range(8):
                            h.MM(P.t[:, 0:w], HT.t[:, k, :], winb.t[:, k, c0:c0 + w], k == 0, k == 7, [HT, winb], [P])
                        h.CP("act" if cg % 2 else "dve", Y.t[:, c0:c0 + w], P.t[:, 0:w], [P], [Y])
                        if cg % 2 == 1:
                            yield
                    h.DMA("sp", proj.t[tt * 128:(tt + 1) * 128, :], Y.t[:], [Y], [proj])
                run_window([(lambda tt=tt: p1_tile(tt)) for tt in range(nt)], 2)
                kb.barrier()


        if upto >= 2 and 2 not in skip:
            with ExitStack() as pes:
                kb.es = pes
                cw = h.tile("cw", [128, 4, 1536], F32)
                dtb = h.tile("dtb", [128, 4], F32)
                nA = h.tile("nA", [128, 4], F32)
                dgain = h.tile("dgain", [128, 128], F32)
                sel4 = h.tile("sel4", [4, 4, 128], F32)
                BD = [h.tile("BD%d" % k, [128, 128], F32) for k in range(2)]
                DB = [h.tile("DB%d" % k, [128, 128], F32) for k in range(7)]
                h.DMA("sp", cw.t[:].rearrange("p k c -> p (k c)"), dn_conv_w.to_broadcast([128, 6144]), [], [cw])
                h.DMA("sp", dtb.t[:], dn_dt_bias.to_broadcast([128, 4]), [], [dtb])
                h.DMA("sp", nA.t[:], dn_a_log.to_broadcast([128, 4]), [], [nA])
                h.DMA("sp", dgain.t[:], dn_out_gain.to_broadcast([128, 128]), [], [dgain])
                h.ACT(nA.t[:], nA.t[:], AF.Exp, [nA], [nA])
                h.TS("dve", nA.t[:], nA.t[:], -1.0, None, ALU.mult, None, [nA], [nA])
                for hh in range(4):
                    h.CP("dve", sel4.t[:, hh, :], G.identf.t[0:4, hh:hh + 1].to_broadcast([4, 128]), [G.identf], [sel4])
                h.CP("dve", BD[0].t[:], G.identf.t[:], [G.identf], [BD[0]])
                for k in range(7):
                    cur = BD[k % 2]
                    nxt = BD[(k + 1) % 2]
                    b = 2 ** (k + 1)
                    if b == 128:
                        h.CP("dve", nxt.t[:], G.onesf.t[:], [G.onesf], [nxt])
                    else:
                        nb = 128 // b
                        h.ASEL(nxt.t[:].rearrange("p (a b) -> p a b", b=b), G.onesf.t[:].rearrange("p (a b) -> p a b", b=b),
                               [[-b, nb], [0, b]], 0, 1, ALU.is_ge, 0.0, [G.onesf], [nxt])
                        h.ASEL(nxt.t[:].rearrange("p (a b) -> p a b", b=b), nxt.t[:].rearrange("p (a b) -> p a b", b=b),
                               [[b, nb], [0, b]], b - 1, -1, ALU.is_ge, 0.0, [nxt], [nxt])
                    h.TT("dve", DB[k].t[:], nxt.t[:], cur.t[:], ALU.subtract, [nxt, cur], [DB[k]])
                u4 = [[h.tile("u4_%d_%d" % (i, k), [128, 1536], F32) for k in range(4)] for i in range(2)]
                zin = [h.tile("zin%d" % i, [128, 520], F32) for i in range(2)]
                acc = h.tile("acc", [128, 1536], F32)
                tmpc = [h.tile("tmpc%d" % i, [128, 1536], F32) for i in range(2)]
                C2 = []
                for i in range(2):
                    c_ = Ctx()
                    c_.qkv = h.tile("qkv%d" % i, [128, 1536], F32)
                    c_.ssum = h.tile("dssum%d" % i, [128, 8], F32)
                    c_.dgz = h.tile("dgz%d" % i, [128, 512], F32)
                    c_.beta = h.tile("beta%d" % i, [128, 4], F32)
                    c_.gg = h.tile("gg%d" % i, [128, 4], F32)
                    c_.gcs = h.tile("gcs%d" % i, [128, 8], F32)
                    c_.eg = h.tile("eg%d" % i, [128, 4], F32)
                    c_.ekd = h.tile("ekd%d" % i, [128, 4], F32)
                    c_.egl = h.tile("egl%d" % i, [128, 4], F32)
                    c_.be = h.tile("be%d" % i, [128, 4], F32)
                    c_.gcT = h.tile("gcT%d" % i, [4, 128], F32)
                    c_.odn = h.tile("odn%d" % i, [128, 512], BF16)
                    C2.append(c_)
                Sf = [h.tile("Sf%d" % i, [128, 128], F32) for i in range(4)]
                Sb = [h.tile("Sb%d" % i, [128, 128], BF16) for i in range(4)]
                SL = []
                for i in range(2):
                    s_ = Ctx()
                    for nm in ("dec", "decT", "dtmp", "qn", "kn", "qd", "Lf", "us", "ojunk", "on"):
                        setattr(s_, nm, h.tile("%s_%d" % (nm, i), [128, 128], F32))
                    for nm in ("qT", "kT", "qdT", "LT", "attnT", "EkT", "Xs", "Dm", "DmT", "vb", "kbg", "kdec", "wT", "vnew"):
                        setattr(s_, nm, h.tile("%s_%d" % (nm, i), [128, 128], BF16))
                    s_.oss = h.tile("doss_%d" % i, [128, 1], F32)
                    s_.bT = h.ptile("bT_%d" % i, [128, 4, 128], F32)
                    s_.bA = h.ptile("bA_%d" % i, [128, 4, 128], F32)
                    s_.bB = h.ptile("bB_%d" % i, [128, 4, 128], F32)
                    s_.bK = h.ptile("bK_%d" % i, [128, 4, 128], F32)
                    SL.append(s_)
                pgb = SL[0].bK
                for hh in range(4):
                    h.MEMSET("pool", Sf[hh].t[:], 0.0, [Sf[hh]])
                    h.MEMSET("pool", Sb[hh].t[:], 0.0, [Sb[hh]])

                def prologue(n):
                    r0 = n * 128
                    U = u4[n % 2]
                    Z = zin[n % 2]
                    c_ = C2[n % 2]
                    qkv = c_.qkv; ssum = c_.ssum; beta = c_.beta; gg = c_.gg; gcs = c_.gcs
                    for k in range(4):
                        sh = 3 - k
                        if n == 0 and sh > 0:
                            h.MEMSET("pool", U[k].t[:], 0.0, [U[k]])
                            h.DMA("sp", U[k].t[sh:128, :], proj.t[0:128 - sh, 0:1536], [proj], [U[k]])
                        else:
                            h.DMA("sp", U[k].t[:], proj.t[r0 - sh:r0 - sh + 128, 0:1536], [proj], [U[k]])
                    h.DMA("sp", Z.t[:], proj.t[r0:r0 + 128, 1536:2056], [proj], [Z])
                    h.TT("dve", acc.t[:], U[3].t[:], cw.t[:, 3, :], ALU.mult, [U[3], cw], [acc])
                    yield
                    for k in range(3):
                        T_ = tmpc[k % 2]
                        h.TT("pool", T_.t[:], U[k].t[:], cw.t[:, k, :], ALU.mult, [U[k], cw], [T_])
                        h.TT("dve", acc.t[:], acc.t[:], T_.t[:], ALU.add, [acc, T_], [acc])
                        yield
                    h.ACT(qkv.t[:], acc.t[:], AF.Silu, [acc], [qkv])
                    h.ACT(c_.dgz.t[:], Z.t[:, 0:512], AF.Silu, [Z], [c_.dgz])
                    h.ACT(beta.t[:], Z.t[:, 512:516], AF.Sigmoid, [Z], [beta])
                    h.TT("pool", c_.dgz.t[:].rearrange("p (a b) -> p a b", b=128), c_.dgz.t[:].rearrange("p (a b) -> p a b", b=128),
                         dgain.t[:].unsqueeze(1).to_broadcast([128, 4, 128]), ALU.mult, [c_.dgz, dgain], [c_.dgz])
                    yield
                    h.TT("dve", acc.t[:, 0:1024], qkv.t[:, 0:1024], qkv.t[:, 0:1024], ALU.mult, [qkv], [acc])
                    kb.op("dve", lambda e, ssum=ssum, acc=acc: e.tensor_reduce(out=ssum.t[:], in_=acc.t[:, 0:1024].rearrange("p (a b) -> p a b", b=128),
                                                                           axis=AX.X, op=ALU.add), bl([acc]), bl([ssum]))
                    h.ACT(ssum.t[:], ssum.t[:], AF.Ln, [ssum], [ssum], bias=EPS)
                    h.ACT(ssum.t[:], ssum.t[:], AF.Exp, [ssum], [ssum], scale=-0.5)
                    h.TS("dve", ssum.t[:, 0:4], ssum.t[:, 0:4], 128.0 ** -0.5, None, ALU.mult, None, [ssum], [ssum])
                    yield
                    h.TT("dve", gg.t[:], Z.t[:, 516:520], dtb.t[:], ALU.add, [Z, dtb], [gg])
                    h.ACT(gg.t[:], gg.t[:], AF.Exp, [gg], [gg])
                    h.ACT(gg.t[:], gg.t[:], AF.Ln, [gg], [gg], bias=1.0)
                    h.TT("dve", gg.t[:], gg.t[:], nA.t[:], ALU.mult, [gg, nA], [gg])
                    h.MM(pgb.t[:, 3, 0:4], G.m_ui.t[:], gg.t[:], True, True, [G.m_ui, gg], [pgb])
                    h.MM(pgb.t[:, 3, 4:8], G.onesf.t[:], gg.t[:], True, True, [G.onesf, gg], [pgb])
                    h.CP("dve", gcs.t[:], pgb.t[:, 3, 0:8], [pgb], [gcs])
                    yield
                    h.ACT(c_.eg.t[:], gcs.t[:, 0:4], AF.Exp, [gcs], [c_.eg])
                    h.ACT(c_.egl.t[:], gcs.t[:, 4:8], AF.Exp, [gcs], [c_.egl])
                    h.TT("dve", c_.ekd.t[:], gcs.t[:, 4:8], gcs.t[:, 0:4], ALU.subtract, [gcs], [c_.ekd])
                    h.ACT(c_.ekd.t[:], c_.ekd.t[:], AF.Exp, [c_.ekd], [c_.ekd])
                    h.TT("dve", c_.be.t[:], beta.t[:], c_.eg.t[:], ALU.mult, [beta, c_.eg], [c_.be])
                    h.TR(pgb.t[0:4, 3, 0:128], gcs.t[:, 0:4], G.identf.t[:], [gcs, G.identf], [pgb])
                    h.CP("dve", c_.gcT.t[:], pgb.t[0:4, 3, 0:128], [pgb], [c_.gcT])
                    yield

                def head(n, hh, sl):
                    c_ = C2[n % 2]
                    s_ = SL[sl]
                    qkv = c_.qkv; ssum = c_.ssum; beta = c_.beta; gcs = c_.gcs
                    qs = qkv.t[:, hh * 128:(hh + 1) * 128]
                    ks = qkv.t[:, 512 + hh * 128:512 + (hh + 1) * 128]
                    vs = qkv.t[:, 1024 + hh * 128:1024 + (hh + 1) * 128]
                    gch = gcs.t[:, hh:hh + 1]
                    bT, bA, bB, bK = s_.bT, s_.bA, s_.bB, s_.bK
                    pX = Tl(bA.t[:, 0, :], bA.b); pU = Tl(bA.t[:, 1, :], bA.b); pW = Tl(bA.t[:, 2, :], bA.b); pO = Tl(bA.t[:, 3, :], bA.b)
                    pY = Tl(bB.t[:, 0, :], bB.b); pYT = Tl(bB.t[:, 1, :], bB.b); pS1 = Tl(bB.t[:, 2, :], bB.b); pSn = Tl(bB.t[:, 3, :], bB.b)
                    pR = Tl(bK.t[:, 0, :], bK.b); pk = Tl(bK.t[:, 1, :], bK.b); pKQ = Tl(bK.t[:, 2, :], bK.b)
                    ptr = [Tl(bT.t[:, i, :], bT.b) for i in range(4)]
                    h.TS("dve", s_.qn.t[:], qs, ssum.t[:, hh:hh + 1], None, ALU.mult, None, [qkv, ssum], [s_.qn])
                    h.TS("dve", s_.kn.t[:], ks, ssum.t[:, 4 + hh:5 + hh], None, ALU.mult, None, [qkv, ssum], [s_.kn])
                    h.TS("dve", s_.qd.t[:], s_.qn.t[:], c_.eg.t[:, hh:hh + 1], None, ALU.mult, None, [s_.qn, c_.eg], [s_.qd])
                    h.MM(pR.t[:], sel4.t[:, hh, :], c_.gcT.t[:], True, True, [sel4, c_.gcT], [pR])
                    yield
                    h.TR(ptr[0].t[:], s_.qn.t[:], G.identf.t[:], [s_.qn, G.identf], [ptr[0]])
                    h.TR(ptr[1].t[:], s_.kn.t[:], G.identf.t[:], [s_.kn, G.identf], [ptr[1]])
                    h.TR(ptr[2].t[:], s_.qd.t[:], G.identf.t[:], [s_.qd, G.identf], [ptr[2]])
                    h.CP("act", s_.qT.t[:], ptr[0].t[:], [ptr[0]], [s_.qT])
                    h.CP("act", s_.kT.t[:], ptr[1].t[:], [ptr[1]], [s_.kT])
                    h.CP("act", s_.qdT.t[:], ptr[2].t[:], [ptr[2]], [s_.qdT])
                    yield
                    h.TS("dve", s_.dtmp.t[:], pR.t[:], gch, 0.0, ALU.subtract, ALU.max, [pR, gcs], [s_.dtmp])
                    h.ACT(s_.dec.t[:], s_.dtmp.t[:], AF.Exp, [s_.dtmp], [s_.dec], scale=-1.0)
                    h.TT("pool", s_.dec.t[:], s_.dec.t[:], G.m_li.t[:], ALU.mult, [s_.dec, G.m_li], [s_.dec])
                    h.TS("dve", s_.dtmp.t[:], pR.t[:], gch, 0.0, ALU.subtract, ALU.min, [pR, gcs], [s_.dtmp])
                    h.ACT(s_.decT.t[:], s_.dtmp.t[:], AF.Exp, [s_.dtmp], [s_.decT])
                    h.TT("pool", s_.decT.t[:], s_.decT.t[:], G.m_ui.t[:], ALU.mult, [s_.decT, G.m_ui], [s_.decT])
                    yield
                    h.TS("dve", s_.kbg.t[:], s_.kn.t[:], c_.be.t[:, hh:hh + 1], None, ALU.mult, None, [s_.kn, c_.be], [s_.kbg])
                    h.TS("dve", s_.kdec.t[:], s_.kn.t[:], c_.ekd.t[:, hh:hh + 1], None, ALU.mult, None, [s_.kn, c_.ekd], [s_.kdec])
                    h.TS("dve", s_.vb.t[:], vs, beta.t[:, hh:hh + 1], None, ALU.mult, None, [qkv, beta], [s_.vb])
                    h.MM(pk.t[:], s_.kT.t[:], s_.kT.t[:], True, True, [s_.kT], [pk])
                    h.MM(pKQ.t[:], s_.kT.t[:], s_.qT.t[:], True, True, [s_.kT, s_.qT], [pKQ])
                    yield
                    h.STT("dve", s_.Lf.t[:], pk.t[:], beta.t[:, hh:hh + 1], s_.dec.t[:], ALU.mult, ALU.mult, [pk, beta, s_.dec], [s_.Lf])
                    h.TT("dve", s_.attnT.t[:], pKQ.t[:], s_.decT.t[:], ALU.mult, [pKQ, s_.decT], [s_.attnT])
                    h.TR(ptr[3].t[:], s_.Lf.t[:], G.identf.t[:], [s_.Lf, G.identf], [ptr[3]])
                    h.CP("act", s_.LT.t[:], ptr[3].t[:], [ptr[3]], [s_.LT])
                    yield
                    Dm = s_.Dm; DmT = s_.DmT; EkT = s_.EkT; Xs = s_.Xs; LT = s_.LT
                    h.TT("pool", Dm.t[:], s_.Lf.t[:], DB[0].t[:], ALU.mult, [s_.Lf, DB[0]], [Dm])
                    h.TT("pool", Dm.t[:], G.identf.t[:], Dm.t[:], ALU.subtract, [G.identf, Dm], [Dm])
                    h.TT("dve", DmT.t[:], LT.t[:], DB[0].t[:], ALU.mult, [LT, DB[0]], [DmT])
                    h.TT("dve", DmT.t[:], G.identf.t[:], DmT.t[:], ALU.subtract, [G.identf, DmT], [DmT])
                    yield
                    for k in range(1, 7):
                        h.TT("pool", EkT.t[:], LT.t[:], DB[k].t[:], ALU.mult, [LT, DB[k]], [EkT])
                        h.MM(pX.t[:], EkT.t[:], Dm.t[:], True, True, [EkT, Dm], [pX])
                        h.CP("act", Xs.t[:], pX.t[:], [pX], [Xs])
                        if k < 6:
                            h.MM(pY.t[:], DmT.t[:], Xs.t[:], True, True, [DmT, Xs], [pY])
                        h.MM(pYT.t[:], Xs.t[:], DmT.t[:], True, True, [Xs, DmT], [pYT])
                        if k < 6:
                            h.TT("dve", Dm.t[:], Dm.t[:], pY.t[:], ALU.subtract, [Dm, pY], [Dm])
                        h.TT("dve", DmT.t[:], DmT.t[:], pYT.t[:], ALU.subtract, [DmT, pYT], [DmT])
                        yield
                    h.MM(pU.t[:], DmT.t[:], s_.vb.t[:], True, True, [DmT, s_.vb], [pU])
                    h.MM(pW.t[:], s_.kbg.t[:], DmT.t[:], True, True, [s_.kbg, DmT], [pW])
                    h.CP("act", s_.us.t[:], pU.t[:], [pU], [s_.us])
                    h.CP("act", s_.wT.t[:], pW.t[:], [pW], [s_.wT])
                    yield
                    h.MM(pS1.t[:], s_.wT.t[:], Sb[hh].t[:], True, True, [s_.wT, Sb[hh]], [pS1])
                    h.TT("dve", s_.vnew.t[:], s_.us.t[:], pS1.t[:], ALU.subtract, [s_.us, pS1], [s_.vnew])
                    h.MM(pO.t[:], s_.qdT.t[:], Sb[hh].t[:], True, False, [s_.qdT, Sb[hh]], [pO])
                    h.MM(pO.t[:], s_.attnT.t[:], s_.vnew.t[:], False, True, [s_.attnT, s_.vnew], [pO])
                    h.MM(pSn.t[:], s_.kdec.t[:], s_.vnew.t[:], True, True, [s_.kdec, s_.vnew], [pSn])
                    yield
                    h.STT("dve", Sf[hh].t[:], Sf[hh].t[:], c_.egl.t[:, hh:hh + 1], pSn.t[:], ALU.mult, ALU.add, [Sf[hh], c_.egl, pSn], [Sf[hh]])
                    h.CP("pool", Sb[hh].t[:], Sf[hh].t[:], [Sf[hh]], [Sb[hh]])
                    oss = s_.oss
                    h.ACT(s_.ojunk.t[:], pO.t[:], AF.Square, [pO], [s_.ojunk, oss], accum_out=oss.t[:])
                    h.ACT(oss.t[:], oss.t[:], AF.Ln, [oss], [oss], scale=1.0 / 128, bias=EPS)
                    h.ACT(oss.t[:], oss.t[:], AF.Exp, [oss], [oss], scale=-0.5)
                    h.ACT(s_.on.t[:], pO.t[:], AF.Copy, [pO, oss], [s_.on], scale=oss.t[:])
                    h.TT("pool", c_.odn.t[:, hh * 128:(hh + 1) * 128], s_.on.t[:], c_.dgz.t[:, hh * 128:(hh + 1) * 128], ALU.mult, [s_.on, c_.dgz], [c_.odn])
                    yield

                def drain2(gens):
                    gens = list(gens)
                    while gens:
                        for g_ in list(gens):
                            try:
                                next(g_)
                            except StopIteration:
                                gens.remove(g_)
                drain2([prologue(0)])
                for n in range(nt if DNSTAGE > 0 else 0):
                    extra = [prologue(n + 1)] if n + 1 < nt else []
                    drain2([head(n, 0, 0), head(n, 1, 1)] + extra)
                    drain2([head(n, 2, 0), head(n, 3, 1)])
                    r0 = n * 128
                    h.DMA("sp", mixed.t[r0:r0 + 128, 0:512], C2[n % 2].odn.t[:], [C2[n % 2].odn], [mixed])
                kb.barrier()

        if upto >= 3 and 3 not in skip:
            with ExitStack() as pes:
                kb.es = pes
                gqk = h.tile("gqk", [128, 2, 64], F32)
                gout = h.tile("gout", [128, 64], F32)
                ones512 = h.tile("ones512", [128, 512], BF16)
                pes2 = ExitStack()
                kb.es = pes2
                sin = [h.tile("sin%d" % i, [128, 1536], F32) for i in range(2)]
                sq2 = h.tile("sq2", [128, 1024], F32)
                ssum = h.tile("ssum", [128, 16], F32)
                qkb = h.tile("qkb", [128, 1024], BF16)
                qkst = [h.tile("qkst%d" % i, [128, 8, 128], BF16) for i in range(2)]
                vst = [h.tile("vst%d" % i, [128, 512], BF16) for i in range(2)]
                pT = h.ptile("pT3", [128, 8, 128], BF16)
                h.DMA("sp", gqk.t[:, 0, :], sb_q_gain.to_broadcast([128, 64]), [], [gqk])
                h.DMA("sp", gqk.t[:, 1, :], sb_k_gain.to_broadcast([128, 64]), [], [gqk])
                h.DMA("sp", gout.t[:], sb_out_gain.to_broadcast([128, 64]), [], [gout])
                h.MEMSET("pool", ones512.t[:], 1.0, [ones512])
                for tt in range(nt):
                    X = sin[tt % 2]
                    QS = qkst[tt % 2]
                    VS = vst[tt % 2]
                    h.DMA("sp", X.t[:], proj.t[tt * 128:(tt + 1) * 128, 2056:3592], [proj], [X])
                    h.TT("dve", sq2.t[:], X.t[:, 0:1024], X.t[:, 0:1024], ALU.mult, [X], [sq2])
                    kb.op("dve", lambda e, X=X, ssum=ssum, sq2=sq2: e.tensor_reduce(out=ssum.t[:], in_=sq2.t[:].rearrange("p (a b) -> p a b", b=64),
                                                                 axis=AX.X, op=ALU.add), bl([sq2]), bl([ssum]))
                    h.ACT(ssum.t[:], ssum.t[:], AF.Ln, [ssum], [ssum], scale=1.0 / 64, bias=EPS)
                    h.ACT(ssum.t[:], ssum.t[:], AF.Exp, [ssum], [ssum], scale=-0.5)
                    h.TS("dve", ssum.t[:, 0:8], ssum.t[:, 0:8], 0.125, None, ALU.mult, None, [ssum], [ssum])
                    h.TT("dve", sq2.t[:].rearrange("p (a b) -> p a b", b=64), X.t[:, 0:1024].rearrange("p (a b) -> p a b", b=64),
                         ssum.t[:].unsqueeze(2).to_broadcast([128, 16, 64]), ALU.mult, [X, ssum], [sq2])
                    for qi in range(2):
                        h.TT("dve", qkb.t[:, qi * 512:(qi + 1) * 512].rearrange("p (a b) -> p a b", b=64),
                             sq2.t[:, qi * 512:(qi + 1) * 512].rearrange("p (a b) -> p a b", b=64),
                             gqk.t[:, qi, :].unsqueeze(1).to_broadcast([128, 8, 64]), ALU.mult, [sq2, gqk], [qkb])
                    for k in range(8):
                        h.TR(pT.t[:, k, :], qkb.t[:, k * 128:(k + 1) * 128], G.ident.t[:], [qkb, G.ident], [pT])
                    h.CP("act", QS.t[:], pT.t[:], [pT], [QS])
                    h.DMA("sp", qT_d.t[:, :, tt * 128:(tt + 1) * 128].rearrange("a p t -> p a t"), QS.t[:, 0:4, :], [QS], [qT_d])
                    h.DMA("sp", kT_d.t[:, :, tt * 128:(tt + 1) * 128].rearrange("a p t -> p a t"), QS.t[:, 4:8, :], [QS], [kT_d])
                    h.CP("act", VS.t[:], X.t[:, 1024:1536], [X], [VS])
                    h.DMA("sp", v_d.t[tt * 128:(tt + 1) * 128, :], VS.t[:], [VS], [v_d])
                kb.barrier()
                pes2.close()
                kb.es = pes
                NS = 4
                qT_hp = h.tile("qT_hp", [128, S], BF16)
                kT_hp = h.tile("kT_hp", [128, S], BF16)
                v_hp = h.tile("v_hp", [128, NT, 128], BF16)
                lpg = [h.tile("lpg%d" % sl, [128, 512], F32) for sl in range(NS)]
                ccg = [[h.tile("ccg%d_%d" % (sl, i), [128, 512], F32) for i in range(2)] for sl in range(NS)]
                ezs = [h.tile("ez%d" % sl, [128, 512], F32) for sl in range(NS)]
                zl2 = [h.tile("zl%d" % i, [128, S], F32) for i in range(NS)]
                arow2 = [h.tile("arow%d" % i, [128, S], BF16) for i in range(NS)]
                aT2 = [h.tile("aT%d" % i, [128, 8, 128], BF16) for i in range(NS)]
                ntots = [h.tile("ntot%d" % i, [128, 1], F32) for i in range(NS)]
                osss = [h.tile("oss%d" % i, [128, 1], F32) for i in range(NS)]
                ojunks = [h.tile("ojunk%d" % i, [128, 64], F32) for i in range(NS)]
                osb = [h.tile("osb%d" % i, [128, 128], BF16) for i in range(4)]
                pzs = [h.ptile("pz%d" % sl, [128, 512], F32) for sl in range(NS)]
                pAs = [h.ptile("pA%d" % i, [128, 8, 128], BF16) for i in range(NS)]
                done_cnt = {}

                def sb_head(hp, i, h2, sl):
                    hh = hp * 2 + h2
                    t1 = (i + 1) * 128
                    ngrp = (t1 + 511) // 512
                    OS = osb[(hp * nt + i) % 4]
                    p0 = h2 * 64
                    zl = zl2[sl]; arow = arow2[sl]; A8 = aT2[sl]
                    ntot = ntots[sl]; oss = osss[sl]; ojunk = ojunks[sl]
                    P = pzs[sl]; E = ezs[sl]; LP = lpg[sl]
                    for g in range(ngrp):
                        k0 = g * 512
                        kw = min(512, t1 - k0)
                        CC = ccg[sl][g % 2]
                        h.MM(P.t[:, 0:kw], qT_hp.t[p0:p0 + 64, i * 128:(i + 1) * 128], kT_hp.t[p0:p0 + 64, k0:k0 + kw],
                             True, True, [qT_hp, kT_hp], [P])
                        h.ACT(E.t[:, 0:kw], P.t[:, 0:kw], AF.Exp, [P], [E])
                        h.ACT(LP.t[:, 0:kw], E.t[:, 0:kw], AF.Ln, [E], [LP], bias=1.0)
                        h.TT("dve", zl.t[:, k0:k0 + kw], P.t[:, 0:kw], LP.t[:, 0:kw], ALU.subtract, [P, LP], [zl])
                        if g == ngrp - 1:
                            h.TT("pool", LP.t[:, kw - 128:kw], LP.t[:, kw - 128:kw], G.m_ls.t[:], ALU.mult, [LP, G.m_ls], [LP])
                        PC = ccg[sl][(g - 1) % 2]
                        init = 0.0 if g == 0 else PC.t[:, 511:512]
                        rd = [ones512, LP] + ([PC] if g > 0 else [])
                        kb.op("dve", lambda e, kw=kw, init=init, CC=CC, ones512=ones512, LP=LP: e.tensor_tensor_scan(
                            out=CC.t[:, 0:kw], data0=ones512.t[:, 0:kw], data1=LP.t[:, 0:kw], initial=init,
                            op0=ALU.mult, op1=ALU.add), bl(rd), bl([CC]))
                        h.TT("pool", zl.t[:, k0:k0 + kw], zl.t[:, k0:k0 + kw], CC.t[:, 0:kw], ALU.add, [zl, CC], [zl])
                        yield
                    kwl = t1 - (ngrp - 1) * 512
                    CL = ccg[sl][(ngrp - 1) % 2]
                    h.TS("dve", ntot.t[:], CL.t[:, kwl - 1:kwl], -1.0, None, ALU.mult, None, [CL], [ntot])
                    h.ACT(arow.t[:, 0:t1], zl.t[:, 0:t1], AF.Exp, [zl, ntot], [arow], bias=ntot.t[:])
                    h.TT("pool", arow.t[:, i * 128:t1], arow.t[:, i * 128:t1], G.m_ls.t[:], ALU.mult, [arow, G.m_ls], [arow])
                    yield
                    PA = pAs[sl]
                    PO = P
                    for b0 in range(0, i + 1, 8):
                        nb = min(8, i + 1 - b0)
                        for bb in range(nb):
                            h.TR(PA.t[:, bb, :], arow.t[:, (b0 + bb) * 128:(b0 + bb + 1) * 128], G.ident.t[:], [arow, G.ident], [PA])
                        h.CP("act", A8.t[:, 0:nb, :], PA.t[:, 0:nb, :], [PA], [A8])
                        for bb in range(nb):
                            blk = b0 + bb
                            h.MM(PO.t[:, 0:64], A8.t[:, bb, :], v_hp.t[:, blk, h2 * 64:(h2 + 1) * 64], blk == 0, blk == i, [A8, v_hp], [PO])
                        yield
                    h.ACT(ojunk.t[:], PO.t[:, 0:64], AF.Square, [PO], [ojunk, oss], accum_out=oss.t[:])
                    h.ACT(oss.t[:], oss.t[:], AF.Ln, [oss], [oss], scale=1.0 / 64, bias=EPS)
                    h.ACT(oss.t[:], oss.t[:], AF.Exp, [oss], [oss], scale=-0.5)
                    h.STT("dve", OS.t[:, h2 * 64:(h2 + 1) * 64], PO.t[:, 0:64], oss.t[:], gout.t[:], ALU.mult, ALU.mult, [PO, oss, gout], [OS])
                    key = (hp, i)
                    done_cnt[key] = done_cnt.get(key, 0) + 1
                    if done_cnt[key] == 2:
                        h.DMA("sp", mixed.t[i * 128:(i + 1) * 128, 512 + hp * 128:512 + (hp + 1) * 128], OS.t[:], [OS], [mixed])

                def load_pair(hp):
                    h.DMA("sp", qT_hp.t[:], qT_d.t[hp], [qT_d], [qT_hp])
                    h.DMA("sp", kT_hp.t[:], kT_d.t[hp], [kT_d], [kT_hp])
                    h.DMA("sp", v_hp.t[:], v_d.t[:, hp * 128:(hp + 1) * 128].rearrange("(b p) c -> p b c", p=128), [v_d], [v_hp])

                for hp in range(4):
                    load_pair(hp)
                    order = []
                    lo_, hi_ = 0, nt - 1
                    while lo_ <= hi_:
                        order.append(hi_)
                        if lo_ != hi_:
                            order.append(lo_)
                        lo_ += 1
                        hi_ -= 1
                    tasks = iter([(hp, i, h2) for i in order for h2 in range(2)])
                    active = []
                    free_slots = list(range(NS - 1, -1, -1))
                    while True:
                        while len(active) < NS:
                            t_ = next(tasks, None)
                            if t_ is None:
                                break
                            sl = free_slots.pop()
                            active.append((sb_head(t_[0], t_[1], t_[2], sl), sl))
                        if not active:
                            break
                        for item in list(active):
                            try:
                                next(item[0])
                            except StopIteration:
                                active.remove(item)
                                free_slots.append(item[1])
                kb.barrier()


        if upto >= 4 and 4 not in skip:
            with ExitStack() as pes:
                kb.es = pes
                woutb = h.tile("woutb", [128, 8, D], BF16)
                wqb = h.tile("wqb", [128, 8, 2048], BF16)
                keysT = h.tile("keysT", [128, 16, 128], BF16)
                pes2 = ExitStack()
                kb.es = pes2
                wst = [h.tile("wst4_%d" % i, [128, 8, 512], F32) for i in range(2)]
                pkT = h.ptile("pkT", [128, 4, 128], F32)
                wov = w_out.rearrange("(k p) n -> p k n", p=128)
                wqv = peer_w_query.rearrange("(k p) n -> p k n", p=128)
                for cg in range(6):
                    W_ = wst[cg % 2]
                    if cg < 2:
                        h.DMA("sp", W_.t[:], wov[:, :, cg * 512:(cg + 1) * 512], [], [W_])
                        h.CP("dve" if cg % 2 else "pool", woutb.t[:, :, cg * 512:(cg + 1) * 512], W_.t[:], [W_], [woutb])
                    else:
                        c0 = (cg - 2) * 512
                        h.DMA("sp", W_.t[:], wqv[:, :, c0:c0 + 512], [], [W_])
                        h.CP("dve" if cg % 2 else "pool", wqb.t[:, :, c0:c0 + 512], W_.t[:], [W_], [wqb])
                ksv = peer_sub_keys.rearrange("a n k -> n a k")
                for g4 in range(4):
                    W_ = wst[g4 % 2]
                    ksf = W_.t[:, 0:4, 0:128]
                    h.DMA("sp", ksf, ksv[:, g4 * 4:(g4 + 1) * 4, :], [], [W_])
                    for j in range(4):
                        h.TR(pkT.t[:, j, :], W_.t[:, j, 0:128], G.identf.t[:], [W_, G.identf], [pkT])
                    h.CP("act", keysT.t[:, g4 * 4:(g4 + 1) * 4, :], pkT.t[:], [pkT], [keysT])
                kb.barrier()
                pes2.close()
                kb.es = pes
                mx = [h.tile("mx%d" % i, [128, D], BF16) for i in range(2)]
                xt = [h.tile("xt4_%d" % i, [128, D], F32) for i in range(2)]
                junk_l = [h.tile("junk4_%d" % i, [128, D], F32) for i in range(2)]
                hb_l = [h.tile("hb4_%d" % i, [128, D], BF16) for i in range(2)]
                ss_l = [h.tile("ss4_%d" % i, [128, 1], F32) for i in range(2)]
                rstd_l = [h.tile("rstd4_%d" % i, [128, 1], F32) for i in range(2)]
                h2T_l = [h.tile("h2T%d" % i, [128, 8, 128], BF16) for i in range(2)]
                x1t_l = [h.tile("x1t%d" % i, [128, D], F32) for i in range(2)]
                mT_l = [h.tile("mT%d" % i, [128, 8, 128], BF16) for i in range(2)]
                qTs = [h.tile("qTs%d" % i, [128, 128], BF16) for i in range(2)]
                scl = [h.tile("sc%d" % i, [128, 16, 128], F32) for i in range(2)]
                pT = h.ptile("pT4", [128, 8, 128], BF16)
                pyo = [h.ptile("pyo%d" % i, [128, 512], F32) for i in range(2)]
                pq = [h.ptile("pq%d" % i, [128, 128], F32) for i in range(2)]
                psc = h.ptile("psc", [128, 4, 128], F32)
                wuf = [h.tile("wuf%d" % i, [128, D], F32) for i in range(2)]
                wvf = [h.tile("wvf%d" % i, [128, D], F32) for i in range(2)]
                wub = [h.tile("wub%d" % i, [128, 8, 128], BF16) for i in range(2)]
                wvb = [h.tile("wvb%d" % i, [128, D], BF16) for i in range(2)]
                ptw = [h.ptile("ptw%d" % i, [128, 4, 128], F32) for i in range(2)]

                def prep_load(c_):
                    h.DMA("sp", wuf[c_ % 2].t[:], peer_w_u[c_ * 128:(c_ + 1) * 128, :], [], [wuf[c_ % 2]])
                    h.DMA("sp", wvf[c_ % 2].t[:], peer_w_v[c_ * 128:(c_ + 1) * 128, :], [], [wvf[c_ % 2]])

                def prep_gen():
                    prep_load(0)
                    for c_ in range(NCH):
                        WU = wuf[c_ % 2]; WV = wvf[c_ % 2]; UB = wub[c_ % 2]; VB = wvb[c_ % 2]
                        for hf in range(2):
                            P_ = ptw[hf]
                            for j in range(4):
                                k = hf * 4 + j
                                h.TR(P_.t[:, j, :], WU.t[:, k * 128:(k + 1) * 128], G.identf.t[:], [WU, G.identf], [P_])
                            h.CP("act" if hf else "dve", UB.t[:, hf * 4:(hf + 1) * 4, :], P_.t[:], [P_], [UB])
                        h.DMA("pool", wuT_d.t[c_], UB.t[:], [UB], [wuT_d])
                        h.CP("pool", VB.t[:], WV.t[:], [WV], [VB])
                        h.DMA("pool", wv_d.t[c_], VB.t[:], [VB], [wv_d])
                        yield
                        if c_ + 1 < NCH:
                            prep_load(c_ + 1)
                prep_it = prep_gen() if (upto >= 5 and 5 not in skip) else iter(())
                def p4_tile(tt):
                    for _ in range((NCH + nt - 1) // nt):
                        next(prep_it, None)
                    r0 = tt * 128
                    SC = scl[tt % 2]
                    MXt = mx[tt % 2]
                    h2T = h2T_l[tt % 2]; x1t = x1t_l[tt % 2]; mT = mT_l[tt % 2]
                    X = xt[tt % 2]
                    h.DMA("sp", MXt.t[:], mixed.t[r0:r0 + 128, :], [mixed], [MXt])
                    h.DMA("sp", X.t[:], x[r0:r0 + 128, :], [], [X])
                    for k in range(8):
                        h.TR(pT.t[:, k, :], MXt.t[:, k * 128:(k + 1) * 128], G.ident.t[:], [MXt, G.ident], [pT])
                    h.CP("act", mT.t[:], pT.t[:], [pT], [mT])
                    for hf in range(2):
                        for k in range(8):
                            h.MM(pyo[hf].t[:], mT.t[:, k, :], woutb.t[:, k, hf * 512:(hf + 1) * 512], k == 0, k == 7, [mT, woutb], [pyo[hf]])
                        h.TT("dve", x1t.t[:, hf * 512:(hf + 1) * 512], pyo[hf].t[:], G.gate1.t[:, hf * 512:(hf + 1) * 512], ALU.mult, [pyo[hf], G.gate1], [x1t])
                    h.TT("pool", x1t.t[:], x1t.t[:], X.t[:], ALU.add, [x1t, X], [x1t])
                    h.DMA("sp", x1.t[r0:r0 + 128, :], x1t.t[:], [x1t], [x1])
                    yield
                    norm_mod_T(x1t, G.G2, G.S2, junk_l[tt % 2], hb_l[tt % 2], ss_l[tt % 2], rstd_l[tt % 2], pT, h2T)
                    h.DMA("sp", h2T_d.t[:, :, r0:r0 + 128], h2T.t[:], [h2T], [h2T_d])
                    yield
                    for hp in range(16):
                        PQ = pq[hp % 2]
                        QT = qTs[hp % 2]
                        for k in range(8):
                            h.MM(PQ.t[:], wqb.t[:, k, hp * 128:(hp + 1) * 128], h2T.t[:, k, :], k == 0, k == 7, [wqb, h2T], [PQ])
                        h.CP("act", QT.t[:], PQ.t[:], [PQ], [QT])
                        h.MM(psc.t[:, hp % 4, :], QT.t[:], keysT.t[:, hp, :], True, True, [QT, keysT], [psc])
                        if hp % 4 == 3:
                            h.CP("dve", SC.t[:, hp - 3:hp + 1, :], psc.t[:], [psc], [SC])
                            yield
                    h.DMA("sp", sc_d.t[r0:r0 + 128], SC.t[:], [SC], [sc_d])
                run_window([(lambda tt=tt: p4_tile(tt)) for tt in range(nt)], 2)
                kb.barrier()

        if upto >= 4 and 4 not in skip:
            with ExitStack() as pes:
                kb.es = pes
                sc_halves = [G.mod[0].t[:].rearrange("p (a b) -> p a b", b=128), G.mod[1].t[:].rearrange("p (a b) -> p a b", b=128)]
                sc_buf = kb.buf("sc4b")

                class _SC:
                    pass
                scw = _SC()
                scw.b = sc_buf
                scw.row = lambda hp: sc_halves[hp // 8][:, hp % 8, :]
                svl = [Tl(G.mod[2].t[:, i * 256:(i + 1) * 256].rearrange("p (a b) -> p a b", b=16), kb.buf()) for i in range(2)]
                wk = Tl(G.mod[2].t[:, 512:640], kb.buf())
                wk2 = Tl(G.mod[2].t[:, 640:896], kb.buf())
                cml = [h.tile("cm%d" % i, [128, 8, 16], F32) for i in range(2)]
                zz = h.tile("zz", [128, 8], F32)
                nbl = [h.tile("nbias%d" % i, [128, 8], F32) for i in range(2)]
                sm_l = [h.tile("sm%d" % i, [128, 16, 128], F32) for i in range(1)]
                ee_l = [h.tile("ee%d" % i, [128, 16, 128], F32) for i in range(1)]
                cand = Tl(sm_l[0].t[:].rearrange("p (a c) b -> p a (c b)", a=8), sm_l[0].b)
                ce = Tl(ee_l[0].t[:].rearrange("p (a c) b -> p a (c b)", a=8), ee_l[0].b)
                smh = [Tl(sm_l[0].t[:, 0:8, :], kb.buf()), Tl(sm_l[0].t[:, 8:16, :], kb.buf()),
                       Tl(G.mod[3].t[:].rearrange("p (a b) -> p a b", b=128), kb.buf()),
                       Tl(G.mod[4].t[:].rearrange("p (a b) -> p a b", b=128), kb.buf())]
                eeh = [Tl(ee_l[0].t[:, 0:8, :], kb.buf()), Tl(ee_l[0].t[:, 8:16, :], kb.buf())]
                Gp = h.tile("Gp", [128, 8, 16, 128], BF16)
                OH = h.tile("OH", [128, 8, 16, 128], BF16)
                GpT = h.tile("GpT", [128, 128, 128], BF16)
                OHT = h.tile("OHT", [128, 128, 128], BF16)
                GTt = h.tile("GTt", [128, 128, 128], BF16)
                pTr = [h.ptile("pTr%d" % i, [128, 8, 128], BF16) for i in range(2)]
                pG4 = [h.ptile("pG4_%d" % i, [128, 4, 128], F32) for i in range(2)]

                def genA(tt):
                    r0 = tt * 128
                    sc = scw; sv = svl[tt % 2]; cm = cml[tt % 2]; nbias = nbl[tt % 2]
                    h.DMA("sp", sc_halves[0], sc_d.t[r0:r0 + 128, 0:8, :], [sc_d], [sc])
                    h.DMA("sp", sc_halves[1], sc_d.t[r0:r0 + 128, 8:16, :], [sc_d], [sc])
                    for hp in range(16):
                        kb.op("dve", lambda e, hp=hp, sv=sv, sc=sc: e.max(out=sv.t[:, hp, 0:8], in_=sc.row(hp)), bl([sc]), bl([sv]))
                        kb.op("dve", lambda e, hp=hp, sv=sv, sc=sc, wk=wk: e.match_replace(out=wk.t[:], in_to_replace=sv.t[:, hp, 0:8], in_values=sc.row(hp), imm_value=-1e30),
                              bl([sc, sv]), bl([wk]))
                        kb.op("dve", lambda e, hp=hp, sv=sv, wk=wk: e.max(out=sv.t[:, hp, 8:16], in_=wk.t[:]), bl([wk]), bl([sv]))
                        if hp % 4 == 3:
                            yield
                    sv4 = sv.t[:].rearrange("p (a b) k -> p a b k", b=2)
                    h.TT("dve", cand.t[:].rearrange("p a (i j) -> p a i j", j=16),
                         sv4[:, :, 0, :].unsqueeze(3).to_broadcast([128, 8, 16, 16]),
                         sv4[:, :, 1, :].unsqueeze(2).to_broadcast([128, 8, 16, 16]), ALU.add, [sv], [cand, smh[0], smh[1]])
                    for hh in range(8):
                        kb.op("dve", lambda e, hh=hh, cm=cm, cand=cand: e.max(out=cm.t[:, hh, 0:8], in_=cand.t[:, hh, :]), bl([cand]), bl([cm]))
                        kb.op("dve", lambda e, hh=hh, cm=cm, cand=cand, wk2=wk2: e.match_replace(out=wk2.t[:], in_to_replace=cm.t[:, hh, 0:8], in_values=cand.t[:, hh, :], imm_value=-1e30),
                              bl([cand, cm]), bl([wk2]))
                        kb.op("dve", lambda e, hh=hh, cm=cm, wk2=wk2: e.max(out=cm.t[:, hh, 8:16], in_=wk2.t[:]), bl([wk2]), bl([cm]))
                        if hh % 4 == 3:
                            yield
                    h.TT("dve", ce.t[:], cand.t[:], cm.t[:, :, 0:1].to_broadcast([128, 8, 256]), ALU.subtract, [cand, cm], [ce, eeh[0], eeh[1]])
                    h.ACT(ce.t[:], ce.t[:], AF.Exp, [ce], [ce])
                    h.TT("dve", cand.t[:], cand.t[:], cm.t[:, :, 15:16].to_broadcast([128, 8, 256]), ALU.is_ge, [cand, cm], [cand])
                    h.TT("dve", ce.t[:], ce.t[:], cand.t[:], ALU.mult, [ce, cand], [ce])
                    kb.op("dve", lambda e, zz=zz, ce=ce: e.tensor_reduce(out=zz.t[:], in_=ce.t[:], axis=AX.X, op=ALU.add), bl([ce]), bl([zz]))
                    h.ACT(zz.t[:], zz.t[:], AF.Ln, [zz], [zz])
                    h.TT("dve", nbias.t[:], zz.t[:], cm.t[:, :, 0], ALU.add, [zz, cm], [nbias])
                    h.TS("dve", nbias.t[:], nbias.t[:], -1.0, None, ALU.mult, None, [nbias], [nbias])
                    yield
                    nhalf = 0
                    for hh in range(8):
                        for hf in range(2):
                            sm = smh[nhalf % 4]; ee = eeh[nhalf % 2]
                            xs = [cand] if nhalf < 4 else []
                            xe = [ce] if nhalf < 2 else []
                            nhalf += 1
                            r_ = slice(hf * 8, (hf + 1) * 8)
                            h.TT("pool", sm.t, sv.t[:, 2 * hh, r_].unsqueeze(2).to_broadcast([128, 8, 128]),
                                 sc.row(2 * hh + 1).unsqueeze(1).to_broadcast([128, 8, 128]), ALU.add, [sv, sc], [sm] + xs)
                            h.ACT(ee.t, sm.t, AF.Exp, [sm, nbias], [ee] + xe, bias=nbias.t[:, hh:hh + 1])
                            h.STT("dve", Gp.t[:, hh, r_, :], sm.t, cm.t[:, hh, 15:16], ee.t, ALU.is_ge, ALU.mult, [sm, cm, ee], [Gp])
                        h.TT("dve", OH.t[:, hh, :, :], sc.row(2 * hh).unsqueeze(1).to_broadcast([128, 16, 128]),
                             sv.t[:, 2 * hh, :].unsqueeze(2).to_broadcast([128, 16, 128]), ALU.is_equal, [sc, sv], [OH])
                        yield

                def genT(tt):
                    ntr = 0
                    for (src, dst) in ((Gp, GpT), (OH, OHT)):
                        for g8 in range(16):
                            PT = pTr[ntr % 2]
                            ev_eng = "act" if ntr % 2 == 0 else "dve"
                            ntr += 1
                            for q in range(8):
                                ii = g8 * 8 + q
                                h.TR(PT.t[:, q, :], src.t[:, :, :, ii].rearrange("p a b -> p (a b)"), G.ident.t[:], [src, G.ident], [PT])
                            h.CP(ev_eng, dst.t[:, g8 * 8:(g8 + 1) * 8, :], PT.t[:], [PT], [dst])
                            yield

                def genB(tt):
                    r0 = tt * 128
                    for t4 in range(32):
                        PG = pG4[t4 % 2]
                        for q in range(4):
                            tl = t4 * 4 + q
                            h.MM(PG.t[:, q, :], GpT.t[:, :, tl], OHT.t[:, :, tl], True, True, [GpT, OHT], [PG])
                        tok0 = t4 * 4
                        h.CP("act", GTt.t[:, :, tok0:tok0 + 4], PG.t[:].rearrange("p t c -> p c t"), [PG], [GTt])
                        if t4 % 2 == 1:
                            yield
                    for c4 in range(4):
                        h.DMA("sp", GT_d.t[c4 * 32:(c4 + 1) * 32, :, r0:r0 + 128].rearrange("c i t -> i c t"), GTt.t[:, c4 * 32:(c4 + 1) * 32, :], [GTt], [GT_d])

                def drain(gens):
                    gens = list(gens)
                    while gens:
                        for g_ in list(gens):
                            try:
                                next(g_)
                            except StopIteration:
                                gens.remove(g_)
                drain([genA(0)])
                drain([genT(0)])
                for tt in range(nt):
                    if tt + 1 < nt:
                        drain([genA(tt + 1), genB(tt)])
                        drain([genT(tt + 1)])
                    else:
                        drain([genB(tt)])
                kb.barrier()

        if upto >= 5 and 5 not in skip:
            with ExitStack() as pes:
                kb.es = pes
                TG = 256
                CB = 8
                NB = NCH // CB
                h2g = [h.tile("h2g%d" % i, [128, 8, TG], BF16) for i in range(2)]
                wu8 = [h.tile("wu8_%d" % i, [128, CB, 8, 128], BF16) for i in range(2)]
                wv8 = [h.tile("wv8_%d" % i, [128, CB, D], BF16) for i in range(2)]
                gtc = [h.tile("gtc%d" % i, [128, TG], BF16) for i in range(8)]
                gl = [h.tile("gl%d" % i, [128, TG], F32) for i in range(2)]
                actb = [h.tile("actb%d" % i, [128, TG], BF16) for i in range(10)]
                ysb = [[h.tile("ysb%d_%d" % (a, b), [128, D], F32) for b in range(2)] for a in range(2)]
                x1r = [h.tile("x1r%d" % i, [128, D], F32) for i in range(2)]
                ppre = [h.ptile("ppre%d" % i, [128, 512], F32) for i in range(2)]
                pyy = [[h.ptile("pyy%d_%d" % (a, b), [128, 512], F32) for b in range(2)] for a in range(2)]
                wuv = wuT_d.t.rearrange("c d k e -> d c k e")
                wvv = wv_d.t.rearrange("c e n -> e c n")
                for tp in range(nt // 4):
                    for tg2 in range(2):
                        t0 = (tp * 2 + tg2) * TG
                        h.DMA("sp", h2g[tg2].t[:], h2T_d.t[:, :, t0:t0 + TG], [h2T_d], [h2g[tg2]])
                    its = [(cb, tg2, cl) for cb in range(NB) for tg2 in range(2) for cl in range(CB)]
                    prev_batch = []
                    cur_batch = []
                    n_it = 0

                    def finish(pv):
                        cb, tg2, cl, AB = pv
                        WV = wv8[cb % 2]
                        for t2 in range(2):
                            for dh in range(2):
                                h.MM(pyy[t2][dh].t[:], AB.t[:, t2 * 128:(t2 + 1) * 128], WV.t[:, cl, dh * 512:(dh + 1) * 512],
                                     cl == 0, cl == CB - 1, [AB, WV], [pyy[t2][dh]])
                        if cl == CB - 1:
                            for t2 in range(2):
                                Y = ysb[tg2][t2]
                                for dh in range(2):
                                    if cb == 0:
                                        h.CP("dve", Y.t[:, dh * 512:(dh + 1) * 512], pyy[t2][dh].t[:], [pyy[t2][dh]], [Y])
                                    else:
                                        h.TT("dve", Y.t[:, dh * 512:(dh + 1) * 512], Y.t[:, dh * 512:(dh + 1) * 512], pyy[t2][dh].t[:], ALU.add,
                                             [Y, pyy[t2][dh]], [Y])
                    for (cb, tg2, cl) in its:
                        c_ = cb * CB + cl
                        t0 = (tp * 2 + tg2) * TG
                        WU = wu8[cb % 2]; WV = wv8[cb % 2]
                        if tg2 == 0 and cl == 0:
                            h.DMA("sp", WU.t[:], wuv[:, cb * CB:(cb + 1) * CB], [wuT_d], [WU])
                            h.DMA("pool", WV.t[:], wvv[:, cb * CB:(cb + 1) * CB], [wv_d], [WV])
                        GC = gtc[n_it % 8]
                        h.DMA("sp", GC.t[:], GT_d.t[c_, :, t0:t0 + TG], [GT_d], [GC])
                        PP = ppre[n_it % 2]; GL = gl[n_it % 2]; AB = actb[n_it % 10]
                        H2 = h2g[tg2]
                        for k in range(8):
                            h.MM(PP.t[:, 0:TG], WU.t[:, cl, k, :], H2.t[:, k, :], k == 0, k == 7, [WU, H2], [PP])
                        h.ACT(GL.t[:], PP.t[:, 0:TG], AF.Gelu, [PP], [GL])
                        h.TT("pool", AB.t[:], GL.t[:], GC.t[:], ALU.mult, [GL, GC], [AB])
                        cur_batch.append((cb, tg2, cl, AB))
                        n_it += 1
                        if len(cur_batch) == 4:
                            for pv in prev_batch:
                                finish(pv)
                            prev_batch = cur_batch
                            cur_batch = []
                    for pv in prev_batch + cur_batch:
                        finish(pv)
                    for tg2 in range(2):
                        for t2 in range(2):
                            r0 = (tp * 2 + tg2) * TG + t2 * 128
                            X1 = x1r[t2]; Y = ysb[tg2][t2]
                            h.DMA("sp", X1.t[:], x1.t[r0:r0 + 128, :], [x1], [X1])
                            h.TT("dve", Y.t[:], Y.t[:], G.gate2.t[:], ALU.mult, [Y, G.gate2], [Y])
                            h.TT("pool", Y.t[:], Y.t[:], X1.t[:], ALU.add, [Y, X1], [Y])
                            h.DMA("sp", OUT.t[r0:r0 + 128, :], Y.t[:], [Y], [OUT])
                kb.barrier()

        kb.final_wait("sp")
        kb.emit()
    return nc


_NC_CACHE = {}


def kernel(**inputs):
    n = 8
    if "nc" not in _NC_CACHE:
        _NC_CACHE["nc"] = build()
    nc = _NC_CACHE["nc"]
    x = np.ascontiguousarray(inputs["x"], dtype=np.float32)
    c = np.ascontiguousarray(inputs["c"], dtype=np.float32)
    shared = {}
    for k, v in inputs.items():
        if k in ("x", "c"):
            continue
        a = np.asarray(v, dtype=np.float32)[0]
        if k == "peer_sub_keys":
            a = a.reshape(16, 128, 128)
        elif k == "dn_conv_w":
            a = a.reshape(1, 6144)
        elif a.ndim == 1:
            a = a[None, :]
        shared[k] = np.ascontiguousarray(a)
    in_maps = []
    for b in range(n):
        m = dict(shared)
        m["x"] = x[b]
        m["c"] = c[b:b + 1]
        in_maps.append(m)
    res = run_bass_kernel_spmd(nc, in_maps, core_ids=list(range(n)))
    return np.stack([np.asarray(r["out"], dtype=np.float32) for r in res.results], axis=0)
```
